# Optimizing a Trainium2 kernel written in Bass

```python
import jax, jax.numpy as jnp
from jax import lax
import numpy as np

D_MODEL = 2048
BATCH = 2
SEQ = 8192
DEPTH = 1

PLE_DIM = 256
NORM_EPS = 1e-6
ATT_HEADS = 8
ATT_HEAD_DIM = 128
ATT_WIDTH = ATT_HEADS * ATT_HEAD_DIM
MOBA_BLOCK = 256
MOBA_TOPK = 3
MOBA_Q_CHUNK = 64
SGU_GROUPS = 8
SGU_GROUP_DIM = 128
SGU_WIDTH = SGU_GROUPS * SGU_GROUP_DIM
SGU_CHUNK = 128
PEER_HEADS = 8
PEER_N_KEYS = 128
PEER_N_EXPERTS = PEER_N_KEYS * PEER_N_KEYS
PEER_D_KEY = 256
PEER_HALF = PEER_D_KEY // 2
PEER_TOPK = 16
PEER_TOKEN_CHUNK = 128
IN_WIDTHS = (ATT_WIDTH, ATT_WIDTH, ATT_WIDTH, SGU_WIDTH, SGU_WIDTH, D_MODEL, D_MODEL)
IN_SPLITS = tuple(int(s) for s in np.cumsum(IN_WIDTHS)[:-1])
IN_TOTAL = int(sum(IN_WIDTHS))

kernel_name = "hybrid_moba_gmlp_peer_ple"


def rms_norm(x, gain):
    xf = x.astype(jnp.float32)
    y = xf * lax.rsqrt(jnp.mean(xf * xf, axis=-1, keepdims=True) + NORM_EPS)
    return (y * gain.astype(jnp.float32)).astype(x.dtype)


def layer_norm(x, gain, bias):
    xf = x.astype(jnp.float32)
    mu = jnp.mean(xf, axis=-1, keepdims=True)
    xc = xf - mu
    y = xc * lax.rsqrt(jnp.mean(xc * xc, axis=-1, keepdims=True) + NORM_EPS)
    return (y * gain.astype(jnp.float32) + bias.astype(jnp.float32)).astype(x.dtype)


def moba_attention(q, k, v):
    b, s, h, dh = q.shape
    nb = -(-s // MOBA_BLOCK)
    s_pad = nb * MOBA_BLOCK
    pad = ((0, 0), (0, s_pad - s), (0, 0), (0, 0))
    q, k, v = [jnp.pad(t, pad).transpose(0, 2, 1, 3) for t in (q, k, v)]
    kb = k.reshape(b, h, nb, MOBA_BLOCK, dh)
    vb = v.reshape(b, h, nb, MOBA_BLOCK, dh)
    k_mean = jnp.mean(kb.astype(jnp.float32), axis=3)
    n_sel = min(MOBA_TOPK, nb)
    n_chunks = s_pad // MOBA_Q_CHUNK
    scale = dh ** -0.5
    bi = jnp.arange(b)[:, None, None, None]
    hi = jnp.arange(h)[None, :, None, None]
    sel_len = n_sel * MOBA_BLOCK

    def chunk_fn(c):
        q0 = c * MOBA_Q_CHUNK
        j = q0 // MOBA_BLOCK
        qc = lax.dynamic_slice_in_dim(q, q0, MOBA_Q_CHUNK, axis=2)
        gate = jnp.einsum('bhqd,bhnd->bhqn', qc.astype(jnp.float32), k_mean)
        gate = jnp.where(jnp.arange(nb) < j, gate, -jnp.inf)
        _, sel = lax.top_k(gate, n_sel)
        sel_valid = jnp.arange(n_sel) < j
        ks = kb[bi, hi, sel]
        vs = vb[bi, hi, sel]
        s_sel = jnp.einsum('bhqd,bhqnkd->bhqnk', qc, ks).astype(jnp.float32) * scale
        s_sel = jnp.where(sel_valid[:, None], s_sel, -jnp.inf).reshape(b, h, MOBA_Q_CHUNK, sel_len)
        k_own = lax.dynamic_slice_in_dim(kb, j, 1, axis=2)[:, :, 0]
        v_own = lax.dynamic_slice_in_dim(vb, j, 1, axis=2)[:, :, 0]
        s_own = jnp.einsum('bhqd,bhkd->bhqk', qc, k_own).astype(jnp.float32) * scale
        q_pos = q0 + jnp.arange(MOBA_Q_CHUNK)
        k_pos = j * MOBA_BLOCK + jnp.arange(MOBA_BLOCK)
        s_own = jnp.where(k_pos[None, :] <= q_pos[:, None], s_own, -jnp.inf)
        probs = jax.nn.softmax(jnp.concatenate([s_sel, s_own], axis=-1), axis=-1)
        p_sel = probs[..., :sel_len].reshape(b, h, MOBA_Q_CHUNK, n_sel, MOBA_BLOCK).astype(vb.dtype)
        p_own = probs[..., sel_len:].astype(vb.dtype)
        return (jnp.einsum('bhqnk,bhqnkd->bhqd', p_sel, vs)
                + jnp.einsum('bhqk,bhkd->bhqd', p_own, v_own))

    outs = lax.map(chunk_fn, jnp.arange(n_chunks))
    out = outs.transpose(1, 0, 3, 2, 4).reshape(b, s_pad, h, dh)
    return out[:, :s]


def spatial_gating(u, vg, ln_g, ln_b, w_s, b_s):
    b, s, _ = u.shape
    vg = layer_norm(vg, ln_g, ln_b)
    nc = s // SGU_CHUNK
    vr = vg.reshape(b, nc, SGU_CHUNK, SGU_GROUPS, SGU_GROUP_DIM)
    causal = jnp.tril(jnp.ones((SGU_CHUNK, SGU_CHUNK), dtype=bool))
    w = jnp.where(causal[None], w_s, jnp.zeros_like(w_s))
    mixed = jnp.einsum('gts,bnsgc->bntgc', w, vr) + b_s.T[:, :, None]
    return u * mixed.reshape(b, s, SGU_WIDTH)


def peer(x, w_query, sub_keys, expert_down, expert_up):
    b, s, d = x.shape
    t = b * s
    xf = x.reshape(t, d)
    q = (xf @ w_query).reshape(t, PEER_HEADS, 2, PEER_HALF)
    scores = jnp.einsum('thpk,hpnk->thpn', q, sub_keys).astype(jnp.float32)
    top_s, top_i = lax.top_k(scores, PEER_TOPK)
    cand = top_s[:, :, 0, :, None] + top_s[:, :, 1, None, :]
    cand_id = top_i[:, :, 0, :, None] * PEER_N_KEYS + top_i[:, :, 1, None, :]
    n_cand = PEER_TOPK * PEER_TOPK
    best_s, best_pos = lax.top_k(cand.reshape(t, PEER_HEADS, n_cand), PEER_TOPK)
    expert_id = jnp.take_along_axis(cand_id.reshape(t, PEER_HEADS, n_cand), best_pos, axis=-1)
    gates = jax.nn.softmax(best_s, axis=-1).astype(x.dtype)
    n_e = PEER_HEADS * PEER_TOPK
    nch = t // PEER_TOKEN_CHUNK

    def chunk_fn(args):
        xc, idc, gc = args
        u = expert_down[idc]
        v = expert_up[idc]
        hid = jax.nn.gelu(jnp.einsum('cd,ced->ce', xc, u), approximate=False)
        return jnp.einsum('ce,ced->cd', gc * hid, v)

    y = lax.map(chunk_fn, (xf.reshape(nch, PEER_TOKEN_CHUNK, d),
                           expert_id.reshape(nch, PEER_TOKEN_CHUNK, n_e),
                           gates.reshape(nch, PEER_TOKEN_CHUNK, n_e)))
    return y.reshape(b, s, d)


def setup_inputs(seed: int = 0) -> dict:
    key = jax.random.key(seed)
    ks = jax.random.split(key, 24)
    f32 = jnp.float32
    L, D = DEPTH, D_MODEL

    def nrm(k, shape, scale):
        return jax.random.normal(k, shape, f32) * scale

    def gain(k, shape):
        return 1.0 + 0.02 * jax.random.normal(k, shape, f32)

    return {
        "x": jax.random.normal(ks[0], (BATCH, SEQ, D), f32),
        "p": jax.random.normal(ks[1], (DEPTH, BATCH, SEQ, PLE_DIM), f32),
        "norm_mix_g": gain(ks[2], (L, D)),
        "w_in": nrm(ks[3], (L, D, IN_TOTAL), D ** -0.5),
        "sgu_ln_g": gain(ks[4], (L, SGU_WIDTH)),
        "sgu_ln_b": nrm(ks[5], (L, SGU_WIDTH), 0.02),
        "sgu_w": nrm(ks[6], (L, SGU_GROUPS, SGU_CHUNK, SGU_CHUNK), SGU_CHUNK ** -0.5),
        "sgu_b": gain(ks[7], (L, SGU_GROUPS, SGU_CHUNK)),
        "w_branch_attn": nrm(ks[8], (L, ATT_WIDTH, D), ATT_WIDTH ** -0.5),
        "w_branch_sgu": nrm(ks[9], (L, SGU_WIDTH, D), SGU_WIDTH ** -0.5),
        "w_out": nrm(ks[10], (L, D, D), D ** -0.5),
        "norm_ffn_g": gain(ks[11], (L, D)),
        "peer_w_query": nrm(ks[12], (L, D, PEER_HEADS * PEER_D_KEY), D ** -0.5),
        "peer_sub_keys": nrm(ks[13], (L, PEER_HEADS, 2, PEER_N_KEYS, PEER_HALF), PEER_HALF ** -0.5),
        "peer_down": nrm(ks[14], (L, PEER_N_EXPERTS, D), D ** -0.5),
        "peer_up": nrm(ks[15], (L, PEER_N_EXPERTS, D), PEER_HEADS ** -0.5),
        "norm_ple_g": gain(ks[16], (L, D)),
        "ple_w_proj": nrm(ks[17], (L, PLE_DIM, D), PLE_DIM ** -0.5),
        "ple_w_gate": nrm(ks[18], (L, D, D), D ** -0.5),
        "final_norm_g": gain(ks[19], (D,)),
    }


def reference(x, p, norm_mix_g, w_in, sgu_ln_g, sgu_ln_b, sgu_w, sgu_b, w_branch_attn, w_branch_sgu,
              w_out, norm_ffn_g, peer_w_query, peer_sub_keys, peer_down, peer_up, norm_ple_g,
              ple_w_proj, ple_w_gate, final_norm_g):
    b, s, _ = x.shape
    h = x
    for i in range(DEPTH):
        xn = rms_norm(h, norm_mix_g[i])
        proj = xn @ w_in[i]
        q, k, v, u, vg, g_a, g_b = jnp.split(proj, IN_SPLITS, axis=-1)
        q = q.reshape(b, s, ATT_HEADS, ATT_HEAD_DIM)
        k = k.reshape(b, s, ATT_HEADS, ATT_HEAD_DIM)
        v = v.reshape(b, s, ATT_HEADS, ATT_HEAD_DIM)
        y_att = moba_attention(q, k, v).reshape(b, s, ATT_WIDTH)
        y_sgu = spatial_gating(jax.nn.gelu(u, approximate=False), jax.nn.gelu(vg, approximate=False),
                               sgu_ln_g[i], sgu_ln_b[i], sgu_w[i], sgu_b[i])
        merged = (jax.nn.sigmoid(g_a) * (y_att @ w_branch_attn[i])
                  + jax.nn.sigmoid(g_b) * (y_sgu @ w_branch_sgu[i]))
        h = h + merged @ w_out[i]
        h = h + peer(rms_norm(h, norm_ffn_g[i]), peer_w_query[i], peer_sub_keys[i], peer_down[i], peer_up[i])
        ple_gate = jax.nn.sigmoid(rms_norm(h, norm_ple_g[i]) @ ple_w_gate[i])
        h = h + ple_gate * (p[i] @ ple_w_proj[i])
    return rms_norm(h, final_norm_g)
```

```python
import contextlib
import os
import numpy as np
import ml_dtypes
import concourse.bass as bass
import concourse.mybir as mybir
from concourse.bass_utils import run_bass_kernel_spmd

F32 = mybir.dt.float32
BF16 = mybir.dt.bfloat16
I32 = mybir.dt.int32
U32 = mybir.dt.uint32
AF = mybir.ActivationFunctionType
ALU = mybir.AluOpType
AX = mybir.AxisListType

ENGS = ("pe", "act", "dve", "pool", "sp")


class Dep:
    __slots__ = ("name", "w", "r")

    def __init__(self, name=""):
        self.name = name
        self.w = {}
        self.r = {}


class Op:
    __slots__ = ("eng", "fn", "waits", "flag", "ckey", "val", "inc")

    def __init__(self, eng, fn, ckey, inc):
        self.eng = eng
        self.fn = fn
        self.waits = []
        self.flag = False
        self.ckey = ckey
        self.val = None
        self.inc = inc


class Phase:
    def __init__(self, S):
        self.S = S
        self.stack = contextlib.ExitStack()

    def __enter__(self):
        self.stack.__enter__()
        return self

    def __exit__(self, *a):
        if a[0] is None:
            self.S.emit()
        return self.stack.__exit__(*a)

    def sb(self, name, shape, dtype):
        self.S.uid += 1
        return self.stack.enter_context(self.S.nc.sbuf_tensor(f"{name}_{self.S.uid}", list(shape), dtype))

    def ps(self, name, shape, dtype):
        self.S.uid += 1
        return self.stack.enter_context(self.S.nc.psum_tensor(f"{name}_{self.S.uid}", list(shape), dtype))


class Sched:
    def __init__(self, nc):
        self.nc = nc
        self.stack = contextlib.ExitStack()
        self.sems = {}
        self.count = {}
        self.ops = {e: [] for e in ENGS}
        self.seen = {e: {} for e in ENGS}
        self.uid = 0
        self.nph = 0
        for e in ENGS[:4]:
            self._sem(e)

    def _sem(self, key):
        if key not in self.sems:
            self.sems[key] = self.stack.enter_context(self.nc.semaphore(f"s_{key}"))
            self.count[key] = 0
        return self.sems[key]

    def phase(self):
        return Phase(self)

    def dep(self, name=""):
        return Dep(name)

    def deps(self, n, name=""):
        return [Dep(f"{name}{i}") for i in range(n)]

    def _add(self, op, reads, writes):
        w = op.waits
        for d in reads:
            for k, o in d.w.items():
                w.append(o)
        for d in writes:
            for k, o in d.w.items():
                if op.eng == "pe" and o.eng == "pe" and not o.ckey.startswith("dma:") and not op.ckey.startswith("dma:"):
                    continue
                w.append(o)
            for k, o in d.r.items():
                w.append(o)
        for d in reads:
            d.r[op.ckey] = op
        for d in writes:
            d.w = {op.ckey: op}
            d.r = {}
        for o in w:
            o.flag = True
        self.ops[op.eng].append(op)

    def op(self, eng, fn, reads=(), writes=()):
        o = Op(eng, fn, eng, 1)
        self._add(o, reads, writes)
        return o

    def dma(self, eng, out, in_, reads=(), writes=(), ch=None, **kw):
        key = "dma:" + ch
        self._sem(key)
        o = Op(eng, lambda e: e.dma_start(out=out, in_=in_, **kw), key, 16)
        o.flag = True
        self._add(o, reads, writes)
        return o

    def emit(self, final=False):
        nc = self.nc
        for e in ENGS:
            ops = self.ops[e]
            last = None
            for o in ops:
                if not o.ckey.startswith("dma:"):
                    last = o
            if last is not None:
                last.flag = True
            for o in ops:
                if o.flag:
                    self.count[o.ckey] += o.inc
                    o.val = self.count[o.ckey]
        totals = dict(self.count)
        sems = self.sems
        seen = self.seen
        ops_all = self.ops

        def run(e, eng):
            sn = seen[e]
            for o in ops_all[e]:
                need = {}
                for p in o.waits:
                    if p.val is None:
                        continue
                    if p.val > need.get(p.ckey, 0):
                        need[p.ckey] = p.val
                for k, v in need.items():
                    if sn.get(k, 0) < v:
                        eng.wait_ge(sems[k], v)
                        sn[k] = v
                ins = o.fn(eng)
                if o.flag:
                    ins.then_inc(sems[o.ckey], o.inc)
            for k, v in totals.items():
                if v > 0 and sn.get(k, 0) < v:
                    eng.wait_ge(sems[k], v)
                    sn[k] = v

        self.nph += 1
        with nc.Block() as block:
            @block.tensor
            def _(eng):
                run("pe", eng)

            @block.scalar
            def _(eng):
                run("act", eng)

            @block.vector
            def _(eng):
                run("dve", eng)

            @block.gpsimd
            def _(eng):
                run("pool", eng)

            @block.sync
            def _(eng):
                run("sp", eng)
        self.ops = {e: [] for e in ENGS}

    def finish(self, deps):
        pass

    def close(self):
        self.stack.close()


D = 2048
NCORE = 8
TOWN = 2048
TALL = 8192
EPS = 1e-6
SCALE = 128 ** -0.5
NEGB = -30000.0
C_Q, C_K, C_V, C_U, C_VG, C_GA, C_GB = 0, 1024, 2048, 3072, 4096, 5120, 7168


class B:
    def __init__(self, S, ph, name, shape, dtype, n, psum=False):
        self.t = [(ph.ps if psum else ph.sb)(f"{name}{i}", shape, dtype) for i in range(n)]
        self.d = [S.dep(f"{name}{i}") for i in range(n)]
        self.n = n
        self.k = -1

    def nxt(self):
        self.k += 1
        i = self.k % self.n
        return self.t[i], self.d[i], i


def build_program(debug_outs=()):
    nc = bass.Bass("TRN2", target_bir_lowering=False)

    def din(name, shape, dt=F32):
        return nc.dram_tensor(name, list(shape), dt, kind="ExternalInput").ap()

    def dscr(name, shape, dt):
        kind = "ExternalOutput" if name in debug_outs else "Internal"
        return nc.dram_tensor(name, list(shape), dt, kind=kind).ap()

    class _Lazy:
        def __init__(self, *a):
            self.a = a
            self.v = None

        def get(self):
            if self.v is None:
                self.v = din(*self.a)
            return self.v

    _x_all = _Lazy("x_all", [TALL, D])
    _x_own = _Lazy("x_own", [TOWN, D])
    _p_own = _Lazy("p_own", [TOWN, 256])
    _pastb_in = _Lazy("pastbias", [1, 8 * 32])
    _ownm_in = _Lazy("ownmask", [1, 8 * 32])
    _cb_in = _Lazy("cbias", [8, 128, 4 * 2 * 256], BF16)
    _ident_in = _Lazy("ident", [128, 128])
    _sgumask_in = _Lazy("sgumask", [128, 128])
    _iota_in = _Lazy("iota", [1, 128])
    _g_mix = _Lazy("norm_mix_g", [1, D])
    _w_in = _Lazy("w_in", [D, 9216])
    _ln_g = _Lazy("sgu_ln_g", [1, 1024])
    _ln_b = _Lazy("sgu_ln_b", [1, 1024])
    _sgu_w = _Lazy("sgu_w", [8, 128, 128])
    _sgu_b = _Lazy("sgu_b", [1, 8 * 128])
    _w_ba = _Lazy("w_branch_attn", [1024, D])
    _w_bs = _Lazy("w_branch_sgu", [1024, D])
    _w_out = _Lazy("w_out", [D, D])
    _g_ffn = _Lazy("norm_ffn_g", [1, D])
    _w_pq = _Lazy("peer_w_query", [D, D])
    _subk = _Lazy("peer_sub_keys", [16, 128, 128])
    _p_down = _Lazy("peer_down", [16384, D])
    _p_up = _Lazy("peer_up", [16384, D])
    _g_ple = _Lazy("norm_ple_g", [1, D])
    _w_pp = _Lazy("ple_w_proj", [256, D])
    _w_pg = _Lazy("ple_w_gate", [D, D])
    _g_fin = _Lazy("final_norm_g", [1, D])
    out = nc.dram_tensor("out", [TOWN, D], F32, kind="ExternalOutput").ap()

    KT_d = dscr("KT_d", [8, 128, TALL], BF16)
    V_d = dscr("V_d", [8, 128, 64, 128], BF16)
    QT_d = dscr("QT_d", [8, 128, TOWN], BF16)
    uT_d = dscr("uT_d", [8, 128, TOWN], BF16)
    sga_d = dscr("sga_d", [16, 128, TOWN], BF16)
    sgb_d = dscr("sgb_d", [16, 128, TOWN], BF16)
    ysgu_d = dscr("ysgu_d", [8, 128, TOWN], BF16)
    yatt_d = dscr("yatt_d", [8, 128, TOWN], BF16)
    h1_d = dscr("h1_d", [TOWN, D], F32)
    hn2T_d = dscr("hn2T_d", [16, 128, TOWN], BF16)
    qpT_d = dscr("qpT_d", [16, 128, TOWN], BF16)
    G_d = dscr("G_d", [16, 128, 128, 128], BF16)
    h2_d = dscr("h2_d", [TOWN, D], F32)

    S = Sched(nc)
    d_KTd = S.deps(16, "KTd")
    d_Vd = S.deps(16, "Vd")
    d_QTd, d_uTd, d_sga, d_sgb, d_ysgu, d_yatt = (S.dep(n) for n in "QTd uTd sga sgb ysgu yatt".split())
    d_h1 = S.deps(16, "h1")
    d_hn2T, d_qpT = S.dep("hn2T"), S.dep("qpT")
    d_G = S.deps(16, "G")
    d_h2 = S.deps(16, "h2")
    d_out = S.dep("out")

    def bc(ap1, n):
        return ap1.to_broadcast([128, n])

    with S.phase() as P0:
        ident_f = P0.sb("ident_f", [128, 128], F32)
        ident_b = P0.sb("ident_b", [128, 128], BF16)
        kmT = P0.sb("kmT", [128, 8, 32], F32)
        kmT_b = P0.sb("kmT_b", [128, 8, 32], BF16)
        d_ident, d_kmT, d_kmTb = S.dep("ident"), S.dep("kmT"), S.dep("kmTb")
        S.dma("sp", ident_f[:], _ident_in.get(), writes=[d_ident], ch="ident")
        S.op("dve", lambda e: e.memset(kmT[:], 0.0), writes=[d_kmT])
        S.op("dve", lambda e: e.tensor_copy(out=ident_b[:], in_=ident_f[:]), reads=[d_ident], writes=[d_ident])

        def rms_norm(ph, src, d_src, gain, d_gain, dst, d_dst, tmp):
            st, d_st = tmp
            S.op("act", lambda e: e.activation(out=dst, in_=src, func=AF.Square, accum_out=st[:, 0:1]),
                 reads=[d_src], writes=[d_dst, d_st])
            S.op("dve", lambda e: e.tensor_scalar(out=st[:, 1:2], in0=st[:, 0:1], scalar1=1.0 / D, scalar2=EPS,
                                                  op0=ALU.mult, op1=ALU.add), reads=[d_st], writes=[d_st])
            S.op("act", lambda e: e.activation(out=st[:, 2:3], in_=st[:, 1:2], func=AF.Sqrt), reads=[d_st], writes=[d_st])
            S.op("dve", lambda e: e.reciprocal(out=st[:, 3:4], in_=st[:, 2:3]), reads=[d_st], writes=[d_st])
            S.op("dve", lambda e: e.scalar_tensor_tensor(out=dst, in0=src, scalar=st[:, 3:4], in1=gain,
                                                         op0=ALU.mult, op1=ALU.mult),
                 reads=[d_src, d_st, d_gain], writes=[d_dst])

        def norm_part1(xsrc, ntiles, gain, d_gain, xin, xn, stt):
            res = []
            for gt in range(ntiles):
                xt, d_xt, k = xin.nxt()
                S.dma("sp", xt[:], xsrc[gt * 128:(gt + 1) * 128, :], writes=[d_xt], ch=f"{id(xin)}_{k}")
                xnt, d_xn, _ = xn.nxt()
                stile, d_stile, _ = stt.nxt()
                rms_norm(None, xt[:], d_xt, gain, d_gain, xnt[:], d_xn, (stile, d_stile))
                res.append((xnt, d_xn))
            return res

        def trans_part2(xns, tp, dst_fn):
            for gt, (xnt, d_xn) in enumerate(xns):
                tpt, d_tp, _ = tp.nxt()
                for dc in range(16):
                    S.op("pe", lambda e, dc=dc, tpt=tpt, xnt=xnt: e.transpose(out=tpt[:, dc, :], in_=xnt[:, dc * 128:(dc + 1) * 128],
                                                                              identity=ident_b[:]),
                         reads=[d_xn, d_ident], writes=[d_tp])
                dst, d_dst = dst_fn(gt)
                S.op("act", lambda e, dst=dst, tpt=tpt: e.activation(out=dst, in_=tpt[:], func=AF.Copy), reads=[d_tp], writes=[d_dst])

        def norm_transpose(ph, xsrc, ntiles, gain, d_gain, xin, xn, tp, stt, dst_fn, src_deps=None):
            tiles = []
            for gt in range(ntiles):
                xt, d_xt, k = xin.nxt()
                tiles.append((xt, d_xt))
                S.dma("sp", xt[:], xsrc[gt * 128:(gt + 1) * 128, :], reads=([src_deps[gt]] if src_deps else []), writes=[d_xt],
                      ch=f"{id(xin)}_{k}")
                xnt, d_xn, _ = xn.nxt()
                stile, d_stile, _ = stt.nxt()
                rms_norm(ph, xt[:], d_xt, gain, d_gain, xnt[:], d_xn, (stile, d_stile))
                tpt, d_tp, _ = tp.nxt()
                for dc in range(16):
                    S.op("pe", lambda e, dc=dc, tpt=tpt, xnt=xnt: e.transpose(out=tpt[:, dc, :], in_=xnt[:, dc * 128:(dc + 1) * 128],
                                                                              identity=ident_b[:]),
                         reads=[d_xn, d_ident], writes=[d_tp])
                dst, d_dst = dst_fn(gt)
                S.op("act", lambda e, dst=dst, tpt=tpt: e.activation(out=dst, in_=tpt[:], func=AF.Copy), reads=[d_tp], writes=[d_dst])
            return tiles

        with (S.phase() if "TOPK" not in debug_outs else contextlib.nullcontext()) as ph:
          if ph is not None:
              wk = ph.sb("wk", [128, 16, 1024], BF16)
              wv = ph.sb("wv", [128, 16, 1024], BF16)
              gbc = ph.sb("gbc", [128, D], F32)
              d_wk, d_wv, d_g = S.deps(2, "wk"), S.deps(2, "wv"), S.dep("g")
              S.dma("sp", gbc[:], bc(_g_mix.get(), D), writes=[d_g], ch="gbc")
              for hf in range(2):
                  S.dma("pool", wk[:, :, hf * 512:(hf + 1) * 512],
                        _w_in.get()[:, C_K + hf * 512:C_K + (hf + 1) * 512].rearrange("(dc p) n -> p dc n", p=128), writes=[d_wk[hf]], ch=f"wk{hf}")
              for hf in range(2):
                  S.dma("pool", wv[:, :, hf * 512:(hf + 1) * 512],
                        _w_in.get()[:, C_V + hf * 512:C_V + (hf + 1) * 512].rearrange("(dc p) n -> p dc n", p=128), writes=[d_wv[hf]], ch=f"wv{hf}")
              xin = B(S, ph, "xin", [128, D], F32, 4)
              xn = B(S, ph, "xn", [128, D], BF16, 4)
              stt = B(S, ph, "stt", [128, 4], F32, 4)
              tp = B(S, ph, "tp", [128, 16, 128], BF16, 2, psum=True)
              xnT = B(S, ph, "xnT", [128, 16, 512], BF16, 2)
              KTs = B(S, ph, "KTs", [128, 8, 512], BF16, 2)
              Vs = B(S, ph, "Vs", [128, 4, 1024], BF16, 2)
              kp = B(S, ph, "kp", [128, 512], F32, 2, psum=True)
              vp = B(S, ph, "vp", [128, 512], F32, 2, psum=True)
              NSTA = int(os.environ.get("KDBG_NSTA", "16"))
              pre = norm_part1(_x_all.get()[0:512, :], 4, gbc[:], d_g, xin, xn, stt)
              for st in range(NSTA):
                  xT, d_xT, _ = xnT.nxt()
                  trans_part2(pre, tp, lambda gt, xT=xT, d_xT=d_xT: (xT[:, :, gt * 128:(gt + 1) * 128], d_xT))
                  if st + 1 < NSTA:
                      pre = norm_part1(_x_all.get()[(st + 1) * 512:(st + 2) * 512, :], 4, gbc[:], d_g, xin, xn, stt)
                  kt, d_kt, kti = KTs.nxt()
                  ADBG = int(os.environ.get("KDBG_A", "9"))
                  for h in range(8 if ADBG >= 2 else 0):
                      pk, d_pk, _ = kp.nxt()
                      for dc in range(16):
                          S.op("pe", lambda e, pk=pk, dc=dc, h=h, xT=xT: e.matmul(pk[:], lhsT=wk[:, dc, h * 128:(h + 1) * 128], rhs=xT[:, dc, :],
                                                                                  start=(dc == 0), stop=(dc == 15)),
                               reads=[d_wk[h // 4], d_xT], writes=[d_pk])
                      for bb in range(2):
                          S.op("act", lambda e, pk=pk, kt=kt, h=h, bb=bb, st=st: e.activation(
                              out=kt[:, h, bb * 256:(bb + 1) * 256], in_=pk[:, bb * 256:(bb + 1) * 256], func=AF.Copy,
                              accum_out=kmT[:, h, st * 2 + bb:st * 2 + bb + 1]), reads=[d_pk], writes=[d_kt, d_kmT])
                  vs, d_vs, vsi = Vs.nxt()
                  for tt in range(4 if ADBG >= 3 else 0):
                      for hf in range(2):
                          pv, d_pv, _ = vp.nxt()
                          for dc in range(16):
                              S.op("pe", lambda e, pv=pv, dc=dc, tt=tt, hf=hf, xT=xT: e.matmul(pv[:], lhsT=xT[:, dc, tt * 128:(tt + 1) * 128],
                                                                                             rhs=wv[:, dc, hf * 512:(hf + 1) * 512],
                                                                                             start=(dc == 0), stop=(dc == 15)),
                                   reads=[d_wv[hf], d_xT], writes=[d_pv])
                          S.op("dve", lambda e, pv=pv, vs=vs, tt=tt, hf=hf: e.tensor_copy(out=vs[:, tt, hf * 512:(hf + 1) * 512], in_=pv[:]),
                               reads=[d_pv], writes=[d_vs])
                  if ADBG >= 4:
                      S.dma("pool", KT_d[:, :, st * 512:(st + 1) * 512].rearrange("h p t -> p h t"), kt[:], reads=[d_kt], writes=[d_KTd[st]], ch=f"KTs{kti}")
                  for tt in range(4 if ADBG >= 5 else 0):
                      S.dma("pool", V_d[:, :, st * 4 + tt, :].rearrange("h p d -> p h d"), vs[:, tt, :].rearrange("p (h d) -> p h d", h=8),
                            reads=[d_vs], writes=[d_Vd[st]], ch=f"Vs{vsi}")
              S.op("dve", lambda e: e.tensor_scalar(out=kmT_b[:], in0=kmT[:], scalar1=1.0 / 256, scalar2=None, op0=ALU.mult),
                   reads=[d_kmT], writes=[d_kmTb])
        if "A" in debug_outs:
            S.close()
            return nc

        def mm(out_, lhsT, rhs, start, stop, reads, writes):
            S.op("pe", lambda e: e.matmul(out_, lhsT=lhsT, rhs=rhs, start=start, stop=stop), reads=reads, writes=writes)


        def proj_fm_g(wsrc, col0, ncols, func, xT, d_xT, dst, d_dst, wb, pp, stg):
            for cg in range(ncols // 512):
                w, d_w, wi = wb.nxt()
                S.dma("pool", w[:], wsrc[:, col0 + cg * 512:col0 + (cg + 1) * 512].rearrange("(dc p) n -> p dc n", p=128),
                      writes=[d_w], ch=f"{id(wb)}_{wi}")
                for fc in range(4):
                    sg, d_sg, si = stg.nxt()
                    for st in range(4):
                        pk, d_pk, _ = pp.nxt()
                        for dc in range(16):
                            mm(pk[:], w[:, dc, fc * 128:(fc + 1) * 128], xT[:, dc, st * 512:(st + 1) * 512], dc == 0, dc == 15,
                               [d_w, d_xT], [d_pk])
                        S.op("act", lambda e, sg=sg, pk=pk, st=st: e.activation(out=sg[:, st * 512:(st + 1) * 512], in_=pk[:], func=func),
                             reads=[d_pk], writes=[d_sg])
                    S.dma("sp", dst[cg * 4 + fc], sg[:], reads=[d_sg], writes=[d_dst], ch=f"{id(stg)}_{si}")


        class TopK:
            def __init__(self, ph, iota_c, d_iota):
                self.iota_c, self.d_iota = iota_c, d_iota
                mk = lambda n, sh, dt, k=1: B(S, ph, n, sh, dt, k)
                self.wk_ = mk("tk_wk", [128, 16, 128], F32)
                self.v16 = mk("tk_v16", [128, 16, 16], F32)
                self.ix16 = mk("tk_ix16", [128, 16, 16], U32)
                self.ixf = mk("tk_ixf", [128, 16, 16], F32)
                self.cand = mk("tk_cand", [128, 8, 256], F32)
                self.cand2 = mk("tk_cand2", [128, 8, 256], F32)
                self.c16 = mk("tk_c16", [128, 8, 16], F32)
                self.pos = mk("tk_pos", [128, 8, 16], U32)
                self.kab = mk("tk_kab", [128, 2, 8, 16], U32)
                self.kabf = mk("tk_kabf", [128, 2, 8, 16], F32)
                self.sm = mk("tk_sm", [128, 3, 8], F32)
                self.e16 = mk("tk_e16", [128, 8, 16], F32)
                self.eq = mk("tk_eq", [128, 8, 16, 16], F32)
                self.res = mk("tk_res", [128, 3, 128], F32)

            def run(self, sc, d_sc):
                nx = lambda b: b.nxt()[:2]
                wk_, d_wk_ = nx(self.wk_)
                v16, d_v = nx(self.v16)
                ix16, d_ix = nx(self.ix16)
                for hp in range(16):
                    S.op("dve", lambda e, hp=hp: e.max(out=v16[:, hp, 0:8], in_=sc[:, hp, :]), reads=[d_sc], writes=[d_v])
                for hp in range(16):
                    S.op("dve", lambda e, hp=hp: e.max_index(out=ix16[:, hp, 0:8], in_max=v16[:, hp, 0:8], in_values=sc[:, hp, :]),
                         reads=[d_sc, d_v], writes=[d_ix])
                for hp in range(16):
                    S.op("dve", lambda e, hp=hp: e.match_replace(out=wk_[:, hp, :], in_to_replace=v16[:, hp, 0:8], in_values=sc[:, hp, :],
                                                                 imm_value=-1e30), reads=[d_sc, d_v], writes=[d_wk_])
                for hp in range(16):
                    S.op("dve", lambda e, hp=hp: e.max(out=v16[:, hp, 8:16], in_=wk_[:, hp, :]), reads=[d_wk_], writes=[d_v])
                for hp in range(16):
                    S.op("dve", lambda e, hp=hp: e.max_index(out=ix16[:, hp, 8:16], in_max=v16[:, hp, 8:16], in_values=wk_[:, hp, :]),
                         reads=[d_wk_, d_v], writes=[d_ix])
                cand, d_c = nx(self.cand)
                cand2, d_c2 = nx(self.cand2)
                c16, d_c16 = nx(self.c16)
                pos, d_pos = nx(self.pos)
                for h in range(8):
                    S.op("dve", lambda e, h=h: e.tensor_tensor(out=cand[:, h, :].rearrange("p (a b) -> p a b", a=16),
                                                               in0=v16[:, 2 * h, :].unsqueeze(2).to_broadcast([128, 16, 16]),
                                                               in1=v16[:, 2 * h + 1, :].unsqueeze(1).to_broadcast([128, 16, 16]), op=ALU.add),
                         reads=[d_v], writes=[d_c])
                for h in range(8):
                    S.op("dve", lambda e, h=h: e.max(out=c16[:, h, 0:8], in_=cand[:, h, :]), reads=[d_c], writes=[d_c16])
                for h in range(8):
                    S.op("dve", lambda e, h=h: e.max_index(out=pos[:, h, 0:8], in_max=c16[:, h, 0:8], in_values=cand[:, h, :]),
                         reads=[d_c, d_c16], writes=[d_pos])
                for h in range(8):
                    S.op("dve", lambda e, h=h: e.match_replace(out=cand2[:, h, :], in_to_replace=c16[:, h, 0:8], in_values=cand[:, h, :],
                                                               imm_value=-1e30), reads=[d_c, d_c16], writes=[d_c2])
                for h in range(8):
                    S.op("dve", lambda e, h=h: e.max(out=c16[:, h, 8:16], in_=cand2[:, h, :]), reads=[d_c2], writes=[d_c16])
                for h in range(8):
                    S.op("dve", lambda e, h=h: e.max_index(out=pos[:, h, 8:16], in_max=c16[:, h, 8:16], in_values=cand2[:, h, :]),
                         reads=[d_c2, d_c16], writes=[d_pos])
                sm, d_sm = nx(self.sm)
                e16, d_e = nx(self.e16)
                res, d_res = nx(self.res)
                S.op("dve", lambda e: e.tensor_scalar(out=sm[:, 0, :], in0=c16[:, :, 0], scalar1=-1.0, scalar2=None, op0=ALU.mult),
                     reads=[d_c16], writes=[d_sm])
                for h in range(8):
                    S.op("act", lambda e, h=h: e.activation(out=e16[:, h, :], in_=c16[:, h, :], func=AF.Exp, bias=sm[:, 0, h:h + 1],
                                                            accum_out=sm[:, 1, h:h + 1]), reads=[d_c16, d_sm], writes=[d_e, d_sm])
                S.op("dve", lambda e: e.reciprocal(out=sm[:, 2, :], in_=sm[:, 1, :]), reads=[d_sm], writes=[d_sm])
                S.op("dve", lambda e: e.tensor_tensor(out=res[:, 2, :].rearrange("p (h k) -> p h k", h=8), in0=e16[:],
                                                      in1=sm[:, 2, :].unsqueeze(2).to_broadcast([128, 8, 16]), op=ALU.mult),
                     reads=[d_e, d_sm], writes=[d_res])
                kab, d_kab = nx(self.kab)
                kabf, d_kabf = nx(self.kabf)
                ixf, d_ixf = nx(self.ixf)
                S.op("dve", lambda e: e.tensor_scalar(out=kab[:, 0], in0=pos[:], scalar1=4, scalar2=None, op0=ALU.logical_shift_right),
                     reads=[d_pos], writes=[d_kab])
                S.op("dve", lambda e: e.tensor_scalar(out=kab[:, 1], in0=pos[:], scalar1=15, scalar2=None, op0=ALU.bitwise_and),
                     reads=[d_pos], writes=[d_kab])
                S.op("dve", lambda e: e.tensor_copy(out=kabf[:], in_=kab[:]), reads=[d_kab], writes=[d_kabf])
                S.op("dve", lambda e: e.tensor_copy(out=ixf[:], in_=ix16[:]), reads=[d_ix], writes=[d_ixf])
                eq, d_eq = nx(self.eq)
                iota16 = self.iota_c[:, 0:16].unsqueeze(1).unsqueeze(1).to_broadcast([128, 8, 16, 16])
                ixv = ixf[:].rearrange("p (h t) k -> p h t k", t=2)
                for t in range(2):
                    S.op("dve", lambda e, t=t: e.tensor_tensor(out=eq[:], in0=kabf[:, t].unsqueeze(3).to_broadcast([128, 8, 16, 16]),
                                                               in1=iota16, op=ALU.is_equal), reads=[d_kabf, self.d_iota], writes=[d_eq])
                    S.op("dve", lambda e, t=t: e.tensor_tensor(out=eq[:], in0=eq[:], in1=ixv[:, :, t, :].unsqueeze(2).to_broadcast([128, 8, 16, 16]),
                                                               op=ALU.mult), reads=[d_eq, d_ixf], writes=[d_eq])
                    S.op("dve", lambda e, t=t: e.tensor_reduce(out=res[:, t, :], in_=eq[:].rearrange("p h k a -> p (h k) a"), axis=AX.X, op=ALU.add),
                         reads=[d_eq], writes=[d_res])
                return res, d_res


        if "TOPK" in debug_outs:
            tk_sc = nc.dram_tensor("tk_sc", [128, 16, 128], F32, kind="ExternalInput").ap()
            tk_out = nc.dram_tensor("tk_out", [128, 3, 128], F32, kind="ExternalOutput").ap()
            with S.phase() as ph:
                iota_c = ph.sb("iota_c", [128, 128], F32)
                sct = ph.sb("sct", [128, 16, 128], F32)
                d_io, d_sct, d_o = S.dep(), S.dep(), S.dep()
                S.dma("sp", iota_c[:], bc(_iota_in.get(), 128), writes=[d_io], ch="iota")
                S.dma("sp", sct[:], tk_sc, writes=[d_sct], ch="sct")
                tk = TopK(ph, iota_c, d_io)
                res, d_res = tk.run(sct, d_sct)
                S.dma("sp", tk_out, res[:], reads=[d_res], writes=[d_o], ch="tko")
            S.close()
            return nc

        with S.phase() as phB:
            xnTo = phB.sb("xnTo", [128, 16, TOWN], BF16)
            d_xnTo = S.dep("xnTo")
            with S.phase() as ph:
                gbc = ph.sb("gbc", [128, D], F32)
                d_g = S.dep("g")
                S.dma("sp", gbc[:], bc(_g_mix.get(), D), writes=[d_g], ch="gbc")
                xin = B(S, ph, "xin", [128, D], F32, 2)
                xn = B(S, ph, "xn", [128, D], BF16, 2)
                stt = B(S, ph, "stt", [128, 4], F32, 2)
                tp = B(S, ph, "tp", [128, 16, 128], BF16, 2, psum=True)
                norm_transpose(ph, _x_own.get(), 16, gbc[:], d_g, xin, xn, tp, stt,
                               lambda gt: (xnTo[:, :, gt * 128:(gt + 1) * 128], d_xnTo))
            with S.phase() as ph:
                wb = B(S, ph, "wb", [128, 16, 512], BF16, 2)
                pp = B(S, ph, "pp", [128, 512], F32, 4, psum=True)
                stg = B(S, ph, "stg", [128, TOWN], BF16, 2)

                def proj_fm(col0, ncols, func, dst, d_dst):
                    proj_fm_g(_w_in.get(), col0, ncols, func, xnTo, d_xnTo, dst, d_dst, wb, pp, stg)

                proj_fm(C_Q, 1024, AF.Copy, QT_d, d_QTd)
                proj_fm(C_U, 1024, AF.Gelu, uT_d, d_uTd)
                proj_fm(C_GA, 2048, AF.Sigmoid, sga_d, d_sga)
                proj_fm(C_GB, 2048, AF.Sigmoid, sgb_d, d_sgb)
            with S.phase() as ph:
                wvg = ph.sb("wvg", [128, 16, 1024], BF16)
                lng = ph.sb("lng", [128, 1024], F32)
                lnb = ph.sb("lnb", [128, 1024], F32)
                bsb = ph.sb("bsb", [128, 1024], F32)
                msk = ph.sb("msk", [128, 128], F32)
                wnat = ph.sb("wnat", [128, 8, 128], F32)
                wsT = ph.sb("wsT", [128, 8, 128], BF16)
                d_ln, d_wsT = S.dep("b2c"), S.dep("wsT")
                d_bsb = d_msk = d_wnat = d_ln
                d_wvg = S.deps(2, "wvg")
                for hf in range(2):
                    S.dma("pool", wvg[:, :, hf * 512:(hf + 1) * 512],
                          _w_in.get()[:, C_VG + hf * 512:C_VG + (hf + 1) * 512].rearrange("(dc p) n -> p dc n", p=128), writes=[d_wvg[hf]], ch=f"wvg{hf}")
                S.dma("sp", lng[:], bc(_ln_g.get(), 1024), writes=[d_ln], ch="b2c")
                S.dma("sp", lnb[:], bc(_ln_b.get(), 1024), writes=[d_ln], ch="b2c")
                S.dma("sp", bsb[:], bc(_sgu_b.get(), 1024), writes=[d_bsb], ch="b2c")
                S.dma("sp", msk[:], _sgumask_in.get(), writes=[d_msk], ch="b2c")
                S.dma("sp", wnat[:], _sgu_w.get().rearrange("g t s -> t g s"), writes=[d_wnat], ch="b2c")
                mp = ph.ps("mp", [128, 8, 128], F32)
                d_mp = S.dep("mp")
                pv2 = B(S, ph, "pv2", [128, 512], F32, 2, psum=True)
                for g in range(8):
                    S.op("pe", lambda e, g=g: e.transpose(out=mp[:, g, :], in_=wnat[:, g, :], identity=ident_f[:]),
                         reads=[d_wnat, d_ident], writes=[d_mp])
                S.op("dve", lambda e: e.tensor_tensor(out=wsT[:], in0=mp[:], in1=msk[:].unsqueeze(1).to_broadcast([128, 8, 128]), op=ALU.mult),
                     reads=[d_mp, d_msk], writes=[d_wsT])
                vga = B(S, ph, "vga", [128, 1024], F32, 2)
                zz = B(S, ph, "zz", [128, 1024], F32, 2)
                vr = B(S, ph, "vr", [128, 1024], BF16, 2)
                ut = B(S, ph, "ut", [128, 8, 128], BF16, 2)
                ys = B(S, ph, "ys", [128, 8, 128], BF16, 2)
                sm = B(S, ph, "sm", [128, 24], F32, 2)
                for tt in range(16):
                    va, d_va, _ = vga.nxt()
                    for hf in range(2):
                        pk, d_pk, _ = pv2.nxt()
                        for dc in range(16):
                            mm(pk[:], xnTo[:, dc, tt * 128:(tt + 1) * 128], wvg[:, dc, hf * 512:(hf + 1) * 512], dc == 0, dc == 15,
                               [d_wvg[hf], d_xnTo], [d_pk])
                        S.op("act", lambda e, va=va, pk=pk, hf=hf: e.activation(out=va[:, hf * 512:(hf + 1) * 512], in_=pk[:], func=AF.Gelu),
                             reads=[d_pk], writes=[d_va])
                    m, d_m, _ = sm.nxt()
                    for hf in range(2):
                        S.op("dve", lambda e, m=m, va=va, hf=hf: e.bn_stats(out=m[:, hf * 6:(hf + 1) * 6], in_=va[:, hf * 512:(hf + 1) * 512]),
                             reads=[d_va], writes=[d_m])
                    S.op("dve", lambda e, m=m: e.bn_aggr(out=m[:, 12:14], in_=m[:, 0:12]), reads=[d_m], writes=[d_m])
                    S.op("dve", lambda e, m=m: e.tensor_scalar(out=m[:, 14:15], in0=m[:, 13:14], scalar1=EPS, scalar2=None, op0=ALU.add),
                         reads=[d_m], writes=[d_m])
                    S.op("act", lambda e, m=m: e.activation(out=m[:, 15:16], in_=m[:, 14:15], func=AF.Sqrt), reads=[d_m], writes=[d_m])
                    S.op("dve", lambda e, m=m: e.reciprocal(out=m[:, 16:17], in_=m[:, 15:16]), reads=[d_m], writes=[d_m])
                    z, d_z, _ = zz.nxt()
                    S.op("dve", lambda e, z=z, va=va, m=m: e.tensor_scalar(out=z[:], in0=va[:], scalar1=m[:, 12:13], scalar2=m[:, 16:17],
                                                                          op0=ALU.subtract, op1=ALU.mult), reads=[d_va, d_m], writes=[d_z])
                    S.op("dve", lambda e, z=z: e.tensor_tensor(out=z[:], in0=z[:], in1=lng[:], op=ALU.mult), reads=[d_z, d_ln], writes=[d_z])
                    v, d_v, _ = vr.nxt()
                    S.op("dve", lambda e, z=z, v=v: e.tensor_tensor(out=v[:], in0=z[:], in1=lnb[:], op=ALU.add), reads=[d_z, d_ln], writes=[d_v])
                    for g in range(8):
                        mm(mp[:, g, :], v[:, g * 128:(g + 1) * 128], wsT[:, g, :], True, True, [d_v, d_wsT], [d_mp])
                    u, d_u, ui = ut.nxt()
                    S.dma("sp", u[:], uT_d[:, :, tt * 128:(tt + 1) * 128].rearrange("g p t -> p g t"), reads=[d_uTd], writes=[d_u], ch=f"ut{ui}")
                    S.op("dve", lambda e, z=z: e.tensor_tensor(out=z[:].rearrange("p (g t) -> p g t", g=8), in0=mp[:],
                                                                in1=bsb[:].rearrange("p (g t) -> p g t", g=8), op=ALU.add),
                         reads=[d_mp, d_bsb, d_v], writes=[d_z])
                    y, d_y, yi = ys.nxt()
                    S.op("dve", lambda e, z=z, y=y, u=u: e.tensor_tensor(out=y[:], in0=z[:].rearrange("p (g t) -> p g t", g=8), in1=u[:], op=ALU.mult),
                         reads=[d_z, d_u], writes=[d_y])
                    S.dma("pool", ysgu_d[:, :, tt * 128:(tt + 1) * 128].rearrange("g p t -> p g t"), y[:], reads=[d_y], writes=[d_ysgu], ch=f"ys{yi}")
        if "B" in debug_outs:
            S.close()
            return nc

        with S.phase() as ph:
            pastb = ph.sb("pastb", [128, 256], F32)
            ownm = ph.sb("ownm", [128, 256], F32)
            d_pm = S.dep("pm")
            S.dma("sp", pastb[:], bc(_pastb_in.get(), 256), writes=[d_pm], ch="pastb")
            S.dma("sp", ownm[:], bc(_ownm_in.get(), 256), writes=[d_pm], ch="pastb")
            KTb = B(S, ph, "KTb", [128, TALL], BF16, 2)
            Vb = B(S, ph, "Vb", [128, 64, 130], BF16, 2)
            for k_ in range(2):
                S.op("pool", lambda e, k_=k_: e.memset(Vb.t[k_][:, :, 128:130], 1.0), writes=[Vb.d[k_]])
            Qb = B(S, ph, "Qb", [128, 256], BF16, 2)
            cbb = B(S, ph, "cbb", [128, 4, 2, 256], BF16, 2)
            spb = B(S, ph, "spb", [128, 512], F32, 3, psum=True)
            ptp = B(S, ph, "ptp", [128, 8, 128], BF16, 2, psum=True)
            opb = B(S, ph, "opb", [128, 512], F32, 2, psum=True)
            gp = ph.ps("gp", [128, 64], F32)
            d_gp = S.dep("gp")
            otp = ptp.t[0][:, 7, :]
            d_otp = ptp.d[0]
            gmb = B(S, ph, "gmb", [128, 32], F32, 2)
            t8b = B(S, ph, "t8b", [128, 10], F32, 2)
            ebb = B(S, ph, "ebb", [128, 32], F32, 2)
            rsb = B(S, ph, "rsb", [128, 2], F32, 2)
            Pb = B(S, ph, "Pb", [128, 512], BF16, 4)
            PTb = B(S, ph, "PTb", [128, 4, 128], BF16, 3)
            Onb = B(S, ph, "Onb", [128, 128], BF16, 2)
            ystb = B(S, ph, "ystb", [128, 256], BF16, 2)
            items = [(i, h) for i in range(int(os.environ.get("KDBG_NI", "8"))) for h in range(8)]
            loaded = {}
            cbl = {}

            def load(idx):
                i, h = items[idx]
                nblk = 4 * i + 4
                if i not in cbl:
                    c_, d_c, ci = cbb.nxt()
                    S.dma("sp", c_[:].rearrange("p a b k -> p (a b k)"), _cb_in.get()[i], writes=[d_c], ch=f"cbb{ci}")
                    cbl[i] = (c_, d_c)
                kt, d_kt, ki = KTb.nxt()
                S.dma("sp", kt[:, 0:nblk * 256], KT_d[h][:, 0:nblk * 256], reads=d_KTd[:nblk // 2], writes=[d_kt], ch=f"KTb{ki}")
                vt, d_vt, vi = Vb.nxt()
                S.dma("sp", vt[:, 0:nblk * 2, 0:128], V_d[h][:, 0:nblk * 2, :], reads=d_Vd[:nblk // 2], writes=[d_vt], ch=f"Vb{vi}")
                qt, d_qt, qi = Qb.nxt()
                S.dma("sp", qt[:], QT_d[h][:, i * 256:(i + 1) * 256], reads=[d_QTd], writes=[d_qt], ch=f"Qb{qi}")
                loaded[idx] = (kt, d_kt, vt, d_vt, qt, d_qt)

            load(0)
            for idx, (i, h) in enumerate(items):
                if idx + 1 < len(items):
                    load(idx + 1)
                kt, d_kt, vt, d_vt, qt, d_qt = loaded.pop(idx)
                cbt, d_cb = cbl[i]
                nblk = 4 * i + 4
                nbp = nblk // 2
                yst, d_yst, ysi = ystb.nxt()
                ebs = []
                for hq in range(2):
                    q = qt[:, hq * 128:(hq + 1) * 128]
                    mm(gp[:, hq * 32:(hq + 1) * 32], q, kmT_b[:, h, :], True, True, [d_qt, d_kmTb], [d_gp])
                    gm, d_gm, _ = gmb.nxt()
                    S.op("dve", lambda e, gm=gm, i=i, hq=hq: e.tensor_tensor(out=gm[:], in0=gp[:, hq * 32:(hq + 1) * 32], in1=pastb[:, i * 32:(i + 1) * 32], op=ALU.add),
                         reads=[d_gp, d_pm], writes=[d_gm])
                    t8, d_t8, _ = t8b.nxt()
                    S.op("dve", lambda e, gm=gm, t8=t8: e.max(out=t8[:, 0:8], in_=gm[:]), reads=[d_gm], writes=[d_t8])
                    S.op("dve", lambda e, t8=t8: e.tensor_scalar(out=t8[:, 8:9], in0=t8[:, 2:3], scalar1=-1e29, scalar2=None, op0=ALU.max),
                         reads=[d_t8], writes=[d_t8])
                    eb, d_eb, _ = ebb.nxt()
                    S.op("dve", lambda e, eb=eb, gm=gm, t8=t8: e.tensor_scalar(out=eb[:], in0=gm[:], scalar1=t8[:, 8:9], scalar2=None, op0=ALU.is_ge),
                         reads=[d_gm, d_t8], writes=[d_eb])
                    S.op("dve", lambda e, eb=eb, i=i: e.tensor_tensor(out=eb[:], in0=eb[:], in1=ownm[:, i * 32:(i + 1) * 32], op=ALU.max),
                         reads=[d_eb, d_pm], writes=[d_eb])
                    S.op("dve", lambda e, eb=eb: e.tensor_scalar(out=eb[:], in0=eb[:], scalar1=-1.0, scalar2=-NEGB, op0=ALU.add, op1=ALU.mult),
                         reads=[d_eb], writes=[d_eb])
                    ebs.append((eb, d_eb))
                for hq in range(2):
                    q = qt[:, hq * 128:(hq + 1) * 128]
                    eb, d_eb = ebs[hq]
                    rs, d_rs, _ = rsb.nxt()
                    o_ps, d_o, _ = opb.nxt()
                    hs = {}

                    def st12(bp):
                        s_t, d_s, _ = spb.nxt()
                        if 2 * bp + 1 < 4 * i:
                            mm(s_t[:], q, kt[:, bp * 512:(bp + 1) * 512], True, True, [d_qt, d_kt], [d_s])
                        else:
                            for nb in range(2):
                                n = 2 * bp + nb
                                causal = n >= 4 * i
                                mm(s_t[:, nb * 256:(nb + 1) * 256], q, kt[:, n * 256:(n + 1) * 256], True, not causal, [d_qt, d_kt], [d_s])
                                if causal:
                                    mm(s_t[:, nb * 256:(nb + 1) * 256], ident_b[:], cbt[:, n - 4 * i, hq, :], False, True, [d_ident, d_cb], [d_s])
                        P, d_P, _ = Pb.nxt()
                        for nb in range(2):
                            n = 2 * bp + nb
                            S.op("act", lambda e, P=P, s_t=s_t, nb=nb, n=n, eb=eb: e.activation(
                                out=P[:, nb * 256:(nb + 1) * 256], in_=s_t[:, nb * 256:(nb + 1) * 256], func=AF.Exp, scale=SCALE,
                                bias=eb[:, n:n + 1]), reads=[d_s, d_eb], writes=[d_P])
                        hs[bp] = [P, d_P]

                    def st34(bp):
                        P, d_P = hs[bp]
                        pp_, d_pp, _ = ptp.nxt()
                        for kk in range(4):
                            S.op("pe", lambda e, pp_=pp_, P=P, kk=kk: e.transpose(out=pp_[:, kk, :], in_=P[:, kk * 128:(kk + 1) * 128], identity=ident_b[:]),
                                 reads=[d_P, d_ident], writes=[d_pp])
                        PT, d_PT, _ = PTb.nxt()
                        S.op("dve", lambda e, PT=PT, pp_=pp_: e.tensor_copy(out=PT[:], in_=pp_[:, 0:4, :]), reads=[d_pp], writes=[d_PT])
                        hs[bp] = [PT, d_PT]

                    def st5(bp):
                        PT, d_PT = hs.pop(bp)
                        for kk in range(4):
                            mm(o_ps[:, 0:130], PT[:, kk, :], vt[:, bp * 4 + kk, :], bp == 0 and kk == 0, bp == nbp - 1 and kk == 3,
                               [d_PT, d_vt], [d_o])

                    for step in range(nbp + 3):
                        if step < nbp:
                            st12(step)
                        if 0 <= step - 2 < nbp:
                            st34(step - 2)
                        if 0 <= step - 3 < nbp:
                            st5(step - 3)
                    S.op("dve", lambda e, rs=rs, o_ps=o_ps: e.reciprocal(out=rs[:, 0:1], in_=o_ps[:, 128:129]), reads=[d_o], writes=[d_rs])
                    On, d_On, _ = Onb.nxt()
                    S.op("act", lambda e, On=On, o_ps=o_ps, rs=rs: e.activation(out=On[:], in_=o_ps[:, 0:128], func=AF.Copy, scale=rs[:, 0:1]),
                         reads=[d_o, d_rs], writes=[d_On])
                    S.op("pe", lambda e, On=On: e.transpose(out=otp, in_=On[:], identity=ident_b[:]), reads=[d_On, d_ident], writes=[d_otp])
                    S.op("dve", lambda e, yst=yst, hq=hq: e.tensor_copy(out=yst[:, hq * 128:(hq + 1) * 128], in_=otp),
                         reads=[d_otp], writes=[d_yst])
                S.dma("pool", yatt_d[h][:, i * 256:(i + 1) * 256], yst[:], reads=[d_yst], writes=[d_yatt], ch=f"yst{ysi}")
        if "C" in debug_outs:
            S.close()
            return nc

        with S.phase() as phD:
            mergedT = phD.sb("mergedT", [128, 16, TOWN], BF16)
            d_mg = S.dep("mergedT")
            with S.phase() as ph:
                yat = ph.sb("yat", [128, 8, TOWN], BF16)
                ysg = ph.sb("ysg", [128, 8, TOWN], BF16)
                d_yat, d_ysg = S.dep("yat"), S.dep("ysg")
                S.dma("sp", yat[:], yatt_d.rearrange("a p t -> p a t"), reads=[d_yatt], writes=[d_yat], ch="yat")
                S.dma("sp", ysg[:], ysgu_d.rearrange("a p t -> p a t"), reads=[d_ysgu], writes=[d_ysg], ch="ysg")
                wab = B(S, ph, "wab", [128, 8, 512], BF16, 2)
                wsb = B(S, ph, "wsb", [128, 8, 512], BF16, 2)
                gab = B(S, ph, "gab", [128, TOWN], BF16, 2)
                gbb = B(S, ph, "gbb", [128, TOWN], BF16, 2)
                pa = B(S, ph, "pa", [128, 512], F32, 2, psum=True)
                pb = B(S, ph, "pb", [128, 512], F32, 2, psum=True)
                t1b = B(S, ph, "t1b", [128, 512], F32, 2)
                t2b = B(S, ph, "t2b", [128, 512], F32, 2)
                for cg in range(4):
                    wa, d_wa, wai = wab.nxt()
                    S.dma("pool", wa[:], _w_ba.get()[:, cg * 512:(cg + 1) * 512].rearrange("(a p) n -> p a n", p=128), writes=[d_wa], ch=f"wab{wai}")
                    ws, d_ws, wsi = wsb.nxt()
                    S.dma("pool", ws[:], _w_bs.get()[:, cg * 512:(cg + 1) * 512].rearrange("(a p) n -> p a n", p=128), writes=[d_ws], ch=f"wsb{wsi}")
                    for fc4 in range(4):
                        fc = cg * 4 + fc4
                        ga, d_ga, gai = gab.nxt()
                        S.dma("sp", ga[:], sga_d[fc], reads=[d_sga], writes=[d_ga], ch=f"gab{gai}")
                        gb, d_gb, gbi = gbb.nxt()
                        S.dma("sp", gb[:], sgb_d[fc], reads=[d_sgb], writes=[d_gb], ch=f"gbb{gbi}")
                        for st in range(4):
                            A, d_A, _ = pa.nxt()
                            for a_ in range(8):
                                mm(A[:], wa[:, a_, fc4 * 128:(fc4 + 1) * 128], yat[:, a_, st * 512:(st + 1) * 512], a_ == 0, a_ == 7, [d_wa, d_yat], [d_A])
                            Sg, d_Sg, _ = pb.nxt()
                            for a_ in range(8):
                                mm(Sg[:], ws[:, a_, fc4 * 128:(fc4 + 1) * 128], ysg[:, a_, st * 512:(st + 1) * 512], a_ == 0, a_ == 7, [d_ws, d_ysg], [d_Sg])
                            T1, d_T1, _ = t1b.nxt()
                            S.op("dve", lambda e, T1=T1, A=A, ga=ga, st=st: e.tensor_tensor(out=T1[:], in0=A[:], in1=ga[:, st * 512:(st + 1) * 512], op=ALU.mult),
                                 reads=[d_A, d_ga], writes=[d_T1])
                            T2, d_T2, _ = t2b.nxt()
                            S.op("dve", lambda e, T2=T2, Sg=Sg, gb=gb, st=st: e.tensor_tensor(out=T2[:], in0=Sg[:], in1=gb[:, st * 512:(st + 1) * 512], op=ALU.mult),
                                 reads=[d_Sg, d_gb], writes=[d_T2])
                            S.op("pool", lambda e, T1=T1, T2=T2, fc=fc, st=st: e.tensor_tensor(out=mergedT[:, fc, st * 512:(st + 1) * 512], in0=T1[:], in1=T2[:], op=ALU.add),
                                 reads=[d_T1, d_T2], writes=[d_mg])
            with S.phase() as ph:
                wo = B(S, ph, "wo", [128, 16, 512], BF16, 2)
                xr = B(S, ph, "xr", [128, 512], F32, 3)
                ho = B(S, ph, "ho", [128, 512], F32, 3)
                po = B(S, ph, "po", [128, 512], F32, 2, psum=True)
                for cg in range(4):
                    w, d_w, wi = wo.nxt()
                    S.dma("pool", w[:], _w_out.get()[:, cg * 512:(cg + 1) * 512].rearrange("(dc p) n -> p dc n", p=128), writes=[d_w], ch=f"wo{wi}")
                    for tt in range(16):
                        x_, d_x, xi = xr.nxt()
                        S.dma("sp", x_[:], _x_own.get()[tt * 128:(tt + 1) * 128, cg * 512:(cg + 1) * 512], writes=[d_x], ch=f"xr{xi}")
                        P, d_P, _ = po.nxt()
                        for fc in range(16):
                            mm(P[:], mergedT[:, fc, tt * 128:(tt + 1) * 128], w[:, fc, :], fc == 0, fc == 15, [d_mg, d_w], [d_P])
                        h_, d_h, hi = ho.nxt()
                        S.op("dve", lambda e, h_=h_, P=P, x_=x_: e.tensor_tensor(out=h_[:], in0=P[:], in1=x_[:], op=ALU.add), reads=[d_P, d_x], writes=[d_h])
                        S.dma("pool", h1_d[tt * 128:(tt + 1) * 128, cg * 512:(cg + 1) * 512], h_[:], reads=[d_h], writes=[d_h1[tt]], ch=f"ho{hi}")
        if "D" in debug_outs:
            S.close()
            return nc

        with S.phase() as phE:
            hn2T = phE.sb("hn2T", [128, 16, TOWN], BF16)
            d_hn = S.dep("hn2T_sb")
            with S.phase() as ph:
                gbc = ph.sb("gbc", [128, D], F32)
                d_g = S.dep("g")
                S.dma("sp", gbc[:], bc(_g_ffn.get(), D), writes=[d_g], ch="gbc2")
                xin = B(S, ph, "xin", [128, D], F32, 2)
                xn = B(S, ph, "xn", [128, D], BF16, 2)
                stt = B(S, ph, "stt", [128, 4], F32, 2)
                tp = B(S, ph, "tp", [128, 16, 128], BF16, 2, psum=True)
                norm_transpose(ph, h1_d, 16, gbc[:], d_g, xin, xn, tp, stt,
                               lambda gt: (hn2T[:, :, gt * 128:(gt + 1) * 128], d_hn), src_deps=d_h1)
                S.dma("sp", hn2T_d.rearrange("c p t -> p c t"), hn2T[:], reads=[d_hn], writes=[d_hn2T], ch="hn2Tst")
            with S.phase() as ph:
                wb = B(S, ph, "wb", [128, 16, 512], BF16, 2)
                pp = B(S, ph, "pp", [128, 512], F32, 4, psum=True)
                stg = B(S, ph, "stg", [128, TOWN], BF16, 2)
                proj_fm_g(_w_pq.get(), 0, 2048, AF.Copy, hn2T, d_hn, qpT_d, d_qpT, wb, pp, stg)
        with S.phase() as ph:
            iota_c = ph.sb("iota_c", [128, 128], F32)
            sknat = ph.sb("sknat", [128, 16, 128], F32)
            skT = ph.sb("skT", [128, 16, 128], BF16)
            d_io, d_sknat, d_skT = S.dep("iota"), S.dep("sknat"), S.dep("skT")
            S.dma("sp", iota_c[:], bc(_iota_in.get(), 128), writes=[d_io], ch="iota")
            S.dma("sp", sknat[:], _subk.get().rearrange("c n k -> n c k"), writes=[d_sknat], ch="sknat")
            scp = ph.ps("scp", [128, 16, 128], F32)
            d_scp = S.dep("scp")
            for hp in range(16):
                S.op("pe", lambda e, hp=hp: e.transpose(out=scp[:, hp, :], in_=sknat[:, hp, :], identity=ident_f[:]), reads=[d_sknat, d_ident], writes=[d_scp])
            S.op("dve", lambda e: e.tensor_copy(out=skT[:], in_=scp[:]), reads=[d_scp], writes=[d_skT])
            tk = TopK(ph, iota_c, d_io)
            qpb = B(S, ph, "qpb", [128, 16, 128], BF16, 3)
            scb = B(S, ph, "scb", [128, 16, 128], F32, 3)
            tr3 = ph.ps("tr3", [128, 4, 128], F32)
            d_tr3 = S.dep("tr3")
            T3b = B(S, ph, "T3b", [128, 3, 128], BF16, 2)
            iota_b = ph.sb("iota_b", [128, 128], BF16)
            S.op("dve", lambda e: e.tensor_copy(out=iota_b[:], in_=iota_c[:]), reads=[d_io], writes=[d_io])
            Pall = B(S, ph, "Pall", [128, 64, 128], BF16, 2)
            Qall = B(S, ph, "Qall", [128, 64, 128], BF16, 2)
            gtp = B(S, ph, "gtp", [128, 4, 128], F32, 2, psum=True)
            GTs = B(S, ph, "GTs", [128, 128, 128], BF16, 1)
            iob = iota_b[:].unsqueeze(1).to_broadcast([128, 64, 128])
            NTE = int(os.environ.get("KDBG_NTE", "16"))

            def e_scores(tt):
                qp, d_qp, qi = qpb.nxt()
                S.dma("sp", qp[:], qpT_d[:, :, tt * 128:(tt + 1) * 128].rearrange("c p t -> p c t"), reads=[d_qpT], writes=[d_qp], ch=f"qpb{qi}")
                for hp in range(16):
                    mm(scp[:, hp, :], qp[:, hp, :], skT[:, hp, :], True, True, [d_qp, d_skT], [d_scp])
                sc, d_sc, _ = scb.nxt()
                S.op("act", lambda e, sc=sc: e.activation(out=sc[:], in_=scp[:], func=AF.Copy), reads=[d_scp], writes=[d_sc])
                return sc, d_sc

            def e_topk(sc, d_sc):
                res, d_res = tk.run(sc, d_sc)
                for t in range(3):
                    S.op("pe", lambda e, t=t, res=res: e.transpose(out=tr3[:, t, :], in_=res[:, t, :], identity=ident_f[:]), reads=[d_res, d_ident], writes=[d_tr3])
                T3, d_T3, _ = T3b.nxt()
                S.op("dve", lambda e, T3=T3: e.tensor_copy(out=T3[:], in_=tr3[:, 0:3, :]), reads=[d_tr3], writes=[d_T3])
                return T3, d_T3

            def e_build(tt, T3, d_T3):
                gts, d_gts, gi = GTs.nxt()
                for half in range(2):
                    P_, d_P, _ = Pall.nxt()
                    Q_, d_Q, _ = Qall.nxt()
                    lo = half * 64
                    S.op("dve", lambda e, P_=P_, T3=T3, lo=lo: e.tensor_tensor(out=P_[:], in0=iob, in1=T3[:, 0, lo:lo + 64].unsqueeze(2).to_broadcast([128, 64, 128]),
                                                                           op=ALU.is_equal), reads=[d_T3, d_io], writes=[d_P])
                    S.op("dve", lambda e, P_=P_, T3=T3, lo=lo: e.tensor_tensor(out=P_[:], in0=P_[:], in1=T3[:, 2, lo:lo + 64].unsqueeze(2).to_broadcast([128, 64, 128]),
                                                                           op=ALU.mult), reads=[d_T3, d_P], writes=[d_P])
                    S.op("dve", lambda e, Q_=Q_, T3=T3, lo=lo: e.tensor_tensor(out=Q_[:], in0=iob, in1=T3[:, 1, lo:lo + 64].unsqueeze(2).to_broadcast([128, 64, 128]),
                                                                           op=ALU.is_equal), reads=[d_T3, d_io], writes=[d_Q])
                    for t4 in range(16):
                        g_, d_g_, _ = gtp.nxt()
                        for u in range(4):
                            tl = t4 * 4 + u
                            mm(g_[:, u, :], Q_[:, tl, :], P_[:, tl, :], True, True, [d_Q, d_P], [d_g_])
                        c0 = lo + t4 * 4
                        S.op("act", lambda e, g_=g_, gts=gts, c0=c0: e.activation(out=gts[:, :, c0:c0 + 4], in_=g_[:].rearrange("p t i -> p i t"), func=AF.Copy),
                             reads=[d_g_], writes=[d_gts])
                S.dma("pool", G_d[tt], gts[:], reads=[d_gts], writes=[d_G[tt]], ch=f"GTs{gi}")

            scs = [e_scores(0)]
            if NTE > 1:
                scs.append(e_scores(1))
            t3s = [e_topk(*scs.pop(0))]
            for tt in range(NTE):
                if tt + 2 < NTE:
                    scs.append(e_scores(tt + 2))
                if tt + 1 < NTE:
                    t3s.append(e_topk(*scs.pop(0)))
                e_build(tt, *t3s.pop(0))
        if "E" in debug_outs:
            S.close()
            return nc

        with S.phase() as ph:
            hnb = B(S, ph, "hnb", [128, 16, 1024], BF16, 1)
            yacc = ph.sb("yacc", [128, 8, D], F32)
            d_yacc = S.deps(8, "yacc")
            dnb = B(S, ph, "dnb", [128, 4, D], BF16, 2)
            upb = B(S, ph, "upb", [128, 4, D], BF16, 2)
            dTb = B(S, ph, "dTb", [128, 4, 16, 128], BF16, 2)
            gtb = B(S, ph, "gtb", [128, 4, 128], BF16, 3)
            geb = B(S, ph, "geb", [128, 4, 128], BF16, 2)
            wtb = B(S, ph, "wtb", [128, 4, 128], BF16, 2)
            tpF = B(S, ph, "tpF", [128, 16, 128], BF16, 1, psum=True)
            hpF = B(S, ph, "hpF", [128, 4, 128], F32, 2, psum=True)
            ypF = B(S, ph, "ypF", [128, 512], F32, 4, psum=True)
            h1t = B(S, ph, "h1t", [128, 1024], F32, 1)
            NSPAN = int(os.environ.get("KDBG_NSPAN", "32"))

            def f_load(s_):
                dn, d_dn, dni = dnb.nxt()
                S.dma("pool", dn[:], _p_down.get()[s_ * 512:(s_ + 1) * 512, :].rearrange("(ic p) d -> p ic d", p=128), writes=[d_dn], ch=f"dnb{dni}")
                up, d_up, upi = upb.nxt()
                S.dma("pool", up[:], _p_up.get()[s_ * 512:(s_ + 1) * 512, :].rearrange("(ic p) d -> p ic d", p=128), writes=[d_up], ch=f"upb{upi}")
                return dn, d_dn, up, d_up

            def f_trans(dn, d_dn):
                dT, d_dT, _ = dTb.nxt()
                for ic in range(4):
                    tpp, d_tpp, _ = tpF.nxt()
                    for dc in range(16):
                        S.op("pe", lambda e, tpp=tpp, dn=dn, ic=ic, dc=dc: e.transpose(out=tpp[:, dc, :], in_=dn[:, ic, dc * 128:(dc + 1) * 128], identity=ident_b[:]),
                             reads=[d_dn, d_ident], writes=[d_tpp])
                    S.op("act", lambda e, tpp=tpp, dT=dT, ic=ic: e.activation(out=dT[:, ic], in_=tpp[:], func=AF.Copy), reads=[d_tpp], writes=[d_dT])
                return dT, d_dT

            def f_hid(g, s_, tt, dT, d_dT, hnt, d_hnt):
                T = g * 8 + tt
                gt, d_gt, gti = gtb.nxt()
                S.dma("sp", gt[:], G_d[T][:, s_ * 4:(s_ + 1) * 4, :], reads=[d_G[T]], writes=[d_gt], ch=f"gtb{gti}")
                hp_, d_hp, _ = hpF.nxt()
                for ic in range(4):
                    for dc in range(16):
                        mm(hp_[:, ic, :], dT[:, ic, dc, :], hnt[:, dc, tt * 128:(tt + 1) * 128], dc == 0, dc == 15, [d_dT, d_hnt], [d_hp])
                ge, d_ge, _ = geb.nxt()
                S.op("act", lambda e, ge=ge, hp_=hp_: e.activation(out=ge[:], in_=hp_[:], func=AF.Gelu), reads=[d_hp], writes=[d_ge])
                wt, d_wt, _ = wtb.nxt()
                S.op("dve", lambda e, wt=wt, ge=ge, gt=gt: e.tensor_tensor(out=wt[:], in0=ge[:], in1=gt[:], op=ALU.mult), reads=[d_ge, d_gt], writes=[d_wt])
                return wt, d_wt

            def f_up(s_, tt, wt, d_wt, up, d_up):
                for c in range(4):
                    yp, d_yp, _ = ypF.nxt()
                    for ic in range(4):
                        mm(yp[:], wt[:, ic, :], up[:, ic, c * 512:(c + 1) * 512], ic == 0, ic == 3, [d_wt, d_up], [d_yp])
                    if s_ == 0:
                        S.op("dve", lambda e, yp=yp, tt=tt, c=c: e.tensor_copy(out=yacc[:, tt, c * 512:(c + 1) * 512], in_=yp[:]),
                             reads=[d_yp], writes=[d_yacc[tt]])
                    else:
                        S.op("dve", lambda e, yp=yp, tt=tt, c=c: e.tensor_tensor(out=yacc[:, tt, c * 512:(c + 1) * 512], in0=yp[:],
                                                                                 in1=yacc[:, tt, c * 512:(c + 1) * 512], op=ALU.add),
                             reads=[d_yp, d_yacc[tt]], writes=[d_yacc[tt]])

            for g in range(2):
                hnt, d_hnt, _ = hnb.nxt()
                S.dma("sp", hnt[:], hn2T_d[:, :, g * 1024:(g + 1) * 1024].rearrange("c p t -> p c t"), reads=[d_hn2T], writes=[d_hnt], ch="hnb")
                ld = f_load(0)
                nxt_ld = f_load(1) if NSPAN > 1 else None
                cur_T = f_trans(ld[0], ld[1])
                pend = None
                for s_ in range(NSPAN):
                    dT, d_dT = cur_T
                    up, d_up = ld[2], ld[3]
                    for tt in range(8):
                        wt, d_wt = f_hid(g, s_, tt, dT, d_dT, hnt, d_hnt)
                        if pend is not None:
                            f_up(*pend)
                        pend = (s_, tt, wt, d_wt, up, d_up)
                        if tt == 0 and s_ >= 1 and s_ + 1 < NSPAN:
                            nxt_ld = f_load(s_ + 1)
                        if tt == 3 and s_ + 1 < NSPAN:
                            nxt_T = f_trans(nxt_ld[0], nxt_ld[1])
                    if s_ + 1 < NSPAN:
                        ld = nxt_ld
                        cur_T = nxt_T
                f_up(*pend)
                for tt in range(8):
                    T = g * 8 + tt
                    for hf in range(2):
                        cs = slice(hf * 1024, (hf + 1) * 1024)
                        h1, d_h1t, _ = h1t.nxt()
                        S.dma("sp", h1[:], h1_d[T * 128:(T + 1) * 128, cs], reads=[d_h1[T]], writes=[d_h1t], ch="h1t")
                        S.op("pool", lambda e, h1=h1, tt=tt, cs=cs: e.tensor_tensor(out=h1[:], in0=h1[:], in1=yacc[:, tt, cs], op=ALU.add),
                             reads=[d_h1t, d_yacc[tt]], writes=[d_h1t])
                        S.dma("sp", h2_d[T * 128:(T + 1) * 128, cs], h1[:], reads=[d_h1t], writes=[d_h2[T]], ch="h1t_out")
        if "F" in debug_outs:
            S.close()
            return nc

        with S.phase() as ph:
            wpg = ph.sb("wpg", [128, 16, D], BF16)
            wpp = ph.sb("wpp", [128, 2, D], BF16)
            gple = ph.sb("gple", [128, D], F32)
            gfin = ph.sb("gfin", [128, D], F32)
            d_wpp, d_gple = S.dep("wpp"), S.dep("gpf")
            d_gfin = d_gple
            d_wpg = S.deps(4, "wpg")
            for cg in range(4):
                S.dma("pool", wpg[:, :, cg * 512:(cg + 1) * 512], _w_pg.get()[:, cg * 512:(cg + 1) * 512].rearrange("(dc p) n -> p dc n", p=128),
                      writes=[d_wpg[cg]], ch=f"wpg{cg}")
            S.dma("pool", wpp[:], _w_pp.get().rearrange("(c p) n -> p c n", p=128), writes=[d_wpp], ch="wpp")
            S.dma("sp", gple[:], bc(_g_ple.get(), D), writes=[d_gple], ch="gple")
            S.dma("sp", gfin[:], bc(_g_fin.get(), D), writes=[d_gfin], ch="gple")
            xin = B(S, ph, "xin", [128, D], F32, 3)
            xn = B(S, ph, "xn", [128, D], BF16, 2)
            stt = B(S, ph, "stt", [128, 4], F32, 2)
            stt2 = B(S, ph, "stt2", [128, 4], F32, 2)
            tp = B(S, ph, "tp", [128, 16, 128], BF16, 1, psum=True)
            hnT = B(S, ph, "hnT", [128, 16, 128], BF16, 2)
            pin = B(S, ph, "pin", [128, 256], F32, 2)
            pbf = B(S, ph, "pbf", [128, 256], BF16, 2)
            pT = B(S, ph, "pT", [128, 2, 128], BF16, 2)
            ptp2 = ph.ps("ptp2", [128, 8, 128], BF16)
            d_ptp2 = S.dep("ptp2")
            gps = B(S, ph, "gps", [128, 512], F32, 2, psum=True)
            pps = B(S, ph, "pps", [128, 512], F32, 2, psum=True)
            sgt = B(S, ph, "sgt", [128, D], F32, 2)
            h3b = B(S, ph, "h3b", [128, D], F32, 2)
            otb = B(S, ph, "otb", [128, D], F32, 2)
            def g_front(tt):
                hT, d_hT, _ = hnT.nxt()
                (xt, d_xt), = norm_transpose(ph, h2_d[tt * 128:(tt + 1) * 128, :], 1, gple[:], d_gple, xin, xn, tp, stt,
                                             lambda gt, hT=hT, d_hT=d_hT: (hT[:], d_hT), src_deps=[d_h2[tt]])
                sg, d_sg, _ = sgt.nxt()
                for c in range(4):
                    G_, d_G_, _ = gps.nxt()
                    for dc in range(16):
                        mm(G_[:], hT[:, dc, :], wpg[:, dc, c * 512:(c + 1) * 512], dc == 0, dc == 15, [d_hT, d_wpg[c]], [d_G_])
                    S.op("act", lambda e, sg=sg, G_=G_, c=c: e.activation(out=sg[:, c * 512:(c + 1) * 512], in_=G_[:], func=AF.Sigmoid), reads=[d_G_], writes=[d_sg])
                pi, d_pi, pii = pin.nxt()
                S.dma("sp", pi[:], _p_own.get()[tt * 128:(tt + 1) * 128, :], writes=[d_pi], ch=f"pin{pii}")
                pb_, d_pb, _ = pbf.nxt()
                S.op("dve", lambda e, pb_=pb_, pi=pi: e.tensor_copy(out=pb_[:], in_=pi[:]), reads=[d_pi], writes=[d_pb])
                for c2 in range(2):
                    S.op("pe", lambda e, pb_=pb_, c2=c2: e.transpose(out=ptp2[:, c2, :], in_=pb_[:, c2 * 128:(c2 + 1) * 128], identity=ident_b[:]),
                         reads=[d_pb, d_ident], writes=[d_ptp2])
                pt_, d_pt, _ = pT.nxt()
                S.op("dve", lambda e, pt_=pt_: e.tensor_copy(out=pt_[:], in_=ptp2[:, 0:2, :]), reads=[d_ptp2], writes=[d_pt])
                return (tt, xt, d_xt, sg, d_sg, pt_, d_pt)

            def g_back(tt, xt, d_xt, sg, d_sg, pt_, d_pt):
                h3, d_h3, _ = h3b.nxt()
                for c in range(4):
                    Pp, d_Pp, _ = pps.nxt()
                    for c2 in range(2):
                        mm(Pp[:], pt_[:, c2, :], wpp[:, c2, c * 512:(c + 1) * 512], c2 == 0, c2 == 1, [d_pt, d_wpp], [d_Pp])
                    S.op("dve", lambda e, h3=h3, Pp=Pp, sg=sg, c=c: e.tensor_tensor(out=h3[:, c * 512:(c + 1) * 512], in0=Pp[:], in1=sg[:, c * 512:(c + 1) * 512], op=ALU.mult),
                         reads=[d_Pp, d_sg], writes=[d_h3])
                S.op("pool", lambda e, h3=h3, xt=xt: e.tensor_tensor(out=h3[:], in0=h3[:], in1=xt[:], op=ALU.add), reads=[d_h3, d_xt], writes=[d_h3])
                ot, d_ot, oti = otb.nxt()
                st2, d_st2, _ = stt2.nxt()
                rms_norm(ph, h3[:], d_h3, gfin[:], d_gfin, ot[:], d_ot, (st2, d_st2))
                S.dma("pool", out[tt * 128:(tt + 1) * 128, :], ot[:], reads=[d_ot], writes=[d_out], ch=f"otb{oti}")

            fr = g_front(0)
            for tt in range(16):
                nfr = g_front(tt + 1) if tt + 1 < 16 else None
                g_back(*fr)
                fr = nfr
    S.close()
    return nc


_PROGRAM = None


def kernel(**inputs):
    global _PROGRAM
    maps, rows_all = make_in_maps(inputs)
    if _PROGRAM is None:
        _PROGRAM = build_program()
    res = run_bass_kernel_spmd(_PROGRAM, maps, core_ids=list(range(NCORE)))
    outp = np.zeros((2, 8192, D), np.float32)
    for c, (b, rows) in enumerate(rows_all):
        outp[b][rows] = np.asarray(res.results[c]["out"]).astype(np.float32)
    return outp


def make_in_maps(inputs):
    f = lambda a: np.ascontiguousarray(np.asarray(a, dtype=np.float32))
    x = f(inputs["x"])
    p = f(inputs["p"])[0]
    ident = np.eye(128, dtype=np.float32)
    sgumask = (np.arange(128)[None, :] >= np.arange(128)[:, None]).astype(np.float32)
    iota = np.arange(128, dtype=np.float32)[None, :]
    shared = {
        "ident": ident, "sgumask": sgumask, "iota": iota,
        "norm_mix_g": f(inputs["norm_mix_g"]).reshape(1, D),
        "w_in": f(inputs["w_in"])[0],
        "sgu_ln_g": f(inputs["sgu_ln_g"]).reshape(1, 1024),
        "sgu_ln_b": f(inputs["sgu_ln_b"]).reshape(1, 1024),
        "sgu_w": f(inputs["sgu_w"])[0],
        "sgu_b": f(inputs["sgu_b"]).reshape(1, 1024),
        "w_branch_attn": f(inputs["w_branch_attn"])[0],
        "w_branch_sgu": f(inputs["w_branch_sgu"])[0],
        "w_out": f(inputs["w_out"])[0],
        "norm_ffn_g": f(inputs["norm_ffn_g"]).reshape(1, D),
        "peer_w_query": f(inputs["peer_w_query"])[0],
        "peer_sub_keys": f(inputs["peer_sub_keys"])[0].reshape(16, 128, 128),
        "peer_down": f(inputs["peer_down"])[0],
        "peer_up": f(inputs["peer_up"])[0],
        "norm_ple_g": f(inputs["norm_ple_g"]).reshape(1, D),
        "ple_w_proj": f(inputs["ple_w_proj"])[0],
        "ple_w_gate": f(inputs["ple_w_gate"])[0],
        "final_norm_g": f(inputs["final_norm_g"]).reshape(1, D),
    }
    maps = []
    rows_all = []
    for c in range(NCORE):
        b, r = c // 4, c % 4
        rows = np.concatenate([np.arange((r + 4 * i) * 256, (r + 4 * i + 1) * 256) for i in range(8)])
        rows_all.append((b, rows))
        pastb = np.zeros((8, 32), np.float32)
        ownm = np.zeros((8, 32), np.float32)
        cb = np.zeros((8, 128, 4, 2, 256), np.float32)
        for i in range(8):
            j = r + 4 * i
            pastb[i, j:] = -1e30
            ownm[i, j] = 1.0
            for hq in range(2):
                qpos = hq * 128 + np.arange(128)[:, None]
                kpos = np.arange(256)[None, :]
                cb[i, :, r, hq, :] = np.where(kpos <= qpos, 0.0, NEGB)
        m = dict(shared)
        m.update({
            "x_all": x[b], "x_own": np.ascontiguousarray(x[b][rows]), "p_own": np.ascontiguousarray(p[b][rows]),
            "pastbias": pastb.reshape(1, 256), "ownmask": ownm.reshape(1, 256),
            "cbias": cb.reshape(8, 128, 2048).astype(ml_dtypes.bfloat16),
        })
        maps.append(m)
    return maps, rows_all
```

```python
import contextlib
import os
import numpy as np
import ml_dtypes
import concourse.bass as bass
import concourse.mybir as mybir
from concourse.bass_utils import run_bass_kernel_spmd

F32 = mybir.dt.float32
BF16 = mybir.dt.bfloat16
I32 = mybir.dt.int32
U32 = mybir.dt.uint32
AF = mybir.ActivationFunctionType
ALU = mybir.AluOpType
AX = mybir.AxisListType

ENGS = ("pe", "act", "dve", "pool", "sp")


class Dep:
    __slots__ = ("name", "w", "r")

    def __init__(self, name=""):
        self.name = name
        self.w = {}
        self.r = {}


class Op:
    __slots__ = ("eng", "fn", "waits", "flag", "ckey", "val", "inc")

    def __init__(self, eng, fn, ckey, inc):
        self.eng = eng
        self.fn = fn
        self.waits = []
        self.flag = False
        self.ckey = ckey
        self.val = None
        self.inc = inc


class Phase:
    def __init__(self, S):
        self.S = S
        self.stack = contextlib.ExitStack()

    def __enter__(self):
        self.stack.__enter__()
        return self

    def __exit__(self, *a):
        if a[0] is None:
            self.S.emit()
        return self.stack.__exit__(*a)

    def sb(self, name, shape, dtype):
        self.S.uid += 1
        return self.stack.enter_context(self.S.nc.sbuf_tensor(f"{name}_{self.S.uid}", list(shape), dtype))

    def ps(self, name, shape, dtype):
        self.S.uid += 1
        return self.stack.enter_context(self.S.nc.psum_tensor(f"{name}_{self.S.uid}", list(shape), dtype))


class Sched:
    def __init__(self, nc):
        self.nc = nc
        self.stack = contextlib.ExitStack()
        self.sems = {}
        self.count = {}
        self.ops = {e: [] for e in ENGS}
        self.seen = {e: {} for e in ENGS}
        self.uid = 0
        self.nph = 0
        for e in ENGS[:4]:
            self._sem(e)

    def _sem(self, key):
        if key not in self.sems:
            self.sems[key] = self.stack.enter_context(self.nc.semaphore(f"s_{key}"))
            self.count[key] = 0
        return self.sems[key]

    def phase(self):
        return Phase(self)

    def dep(self, name=""):
        return Dep(name)

    def deps(self, n, name=""):
        return [Dep(f"{name}{i}") for i in range(n)]

    def _add(self, op, reads, writes):
        w = op.waits
        for d in reads:
            for k, o in d.w.items():
                w.append(o)
        for d in writes:
            for k, o in d.w.items():
                if op.eng == "pe" and o.eng == "pe" and not o.ckey.startswith("dma:") and not op.ckey.startswith("dma:"):
                    continue
                w.append(o)
            for k, o in d.r.items():
                w.append(o)
        for d in reads:
            d.r[op.ckey] = op
        for d in writes:
            d.w = {op.ckey: op}
            d.r = {}
        for o in w:
            o.flag = True
        self.ops[op.eng].append(op)

    def op(self, eng, fn, reads=(), writes=()):
        o = Op(eng, fn, eng, 1)
        self._add(o, reads, writes)
        return o

    def dma(self, eng, out, in_, reads=(), writes=(), ch=None, **kw):
        key = "dma:" + ch
        self._sem(key)
        o = Op(eng, lambda e: e.dma_start(out=out, in_=in_, **kw), key, 16)
        o.flag = True
        self._add(o, reads, writes)
        return o

    def emit(self, final=False):
        nc = self.nc
        for e in ENGS:
            ops = self.ops[e]
            last = None
            for o in ops:
                if not o.ckey.startswith("dma:"):
                    last = o
            if last is not None:
                last.flag = True
            for o in ops:
                if o.flag:
                    self.count[o.ckey] += o.inc
                    o.val = self.count[o.ckey]
        totals = dict(self.count)
        sems = self.sems
        seen = self.seen
        ops_all = self.ops

        def run(e, eng):
            sn = seen[e]
            for o in ops_all[e]:
                need = {}
                for p in o.waits:
                    if p.val is None:
                        continue
                    if p.val > need.get(p.ckey, 0):
                        need[p.ckey] = p.val
                for k, v in need.items():
                    if sn.get(k, 0) < v:
                        eng.wait_ge(sems[k], v)
                        sn[k] = v
                ins = o.fn(eng)
                if o.flag:
                    ins.then_inc(sems[o.ckey], o.inc)
            for k, v in totals.items():
                if v > 0 and sn.get(k, 0) < v:
                    eng.wait_ge(sems[k], v)
                    sn[k] = v

        self.nph += 1
        with nc.Block() as block:
            @block.tensor
            def _(eng):
                run("pe", eng)

            @block.scalar
            def _(eng):
                run("act", eng)

            @block.vector
            def _(eng):
                run("dve", eng)

            @block.gpsimd
            def _(eng):
                run("pool", eng)

            @block.sync
            def _(eng):
                run("sp", eng)
        self.ops = {e: [] for e in ENGS}

    def finish(self, deps):
        pass

    def close(self):
        self.stack.close()


D = 2048
NCORE = 8
TOWN = 2048
TALL = 8192
EPS = 1e-6
SCALE = 128 ** -0.5
NEGB = -30000.0
C_Q, C_K, C_V, C_U, C_VG, C_GA, C_GB = 0, 1024, 2048, 3072, 4096, 5120, 7168


class B:
    def __init__(self, S, ph, name, shape, dtype, n, psum=False):
        self.t = [(ph.ps if psum else ph.sb)(f"{name}{i}", shape, dtype) for i in range(n)]
        self.d = [S.dep(f"{name}{i}") for i in range(n)]
        self.n = n
        self.k = -1

    def nxt(self):
        self.k += 1
        i = self.k % self.n
        return self.t[i], self.d[i], i


def build_program(debug_outs=()):
    nc = bass.Bass("TRN2", target_bir_lowering=False)

    def din(name, shape, dt=F32):
        return nc.dram_tensor(name, list(shape), dt, kind="ExternalInput").ap()

    def dscr(name, shape, dt):
        kind = "ExternalOutput" if name in debug_outs else "Internal"
        return nc.dram_tensor(name, list(shape), dt, kind=kind).ap()

    class _Lazy:
        def __init__(self, *a):
            self.a = a
            self.v = None

        def get(self):
            if self.v is None:
                self.v = din(*self.a)
            return self.v

    _x_all = _Lazy("x_all", [TALL, D])
    _x_own = _Lazy("x_own", [TOWN, D])
    _p_own = _Lazy("p_own", [TOWN, 256])
    _pastb_in = _Lazy("pastbias", [1, 8 * 32])
    _ownm_in = _Lazy("ownmask", [1, 8 * 32])
    _cb_in = _Lazy("cbias", [8, 128, 4 * 2 * 256], BF16)
    _ident_in = _Lazy("ident", [128, 128])
    _sgumask_in = _Lazy("sgumask", [128, 128])
    _iota_in = _Lazy("iota", [1, 128])
    _g_mix = _Lazy("norm_mix_g", [1, D])
    _w_in = _Lazy("w_in", [D, 9216])
    _ln_g = _Lazy("sgu_ln_g", [1, 1024])
    _ln_b = _Lazy("sgu_ln_b", [1, 1024])
    _sgu_w = _Lazy("sgu_w", [8, 128, 128])
    _sgu_b = _Lazy("sgu_b", [1, 8 * 128])
    _w_ba = _Lazy("w_branch_attn", [1024, D])
    _w_bs = _Lazy("w_branch_sgu", [1024, D])
    _w_out = _Lazy("w_out", [D, D])
    _g_ffn = _Lazy("norm_ffn_g", [1, D])
    _w_pq = _Lazy("peer_w_query", [D, D])
    _subk = _Lazy("peer_sub_keys", [16, 128, 128])
    _p_down = _Lazy("peer_down", [16384, D])
    _p_up = _Lazy("peer_up", [16384, D])
    _g_ple = _Lazy("norm_ple_g", [1, D])
    _w_pp = _Lazy("ple_w_proj", [256, D])
    _w_pg = _Lazy("ple_w_gate", [D, D])
    _g_fin = _Lazy("final_norm_g", [1, D])
    out = nc.dram_tensor("out", [TOWN, D], F32, kind="ExternalOutput").ap()

    KT_d = dscr("KT_d", [8, 128, TALL], BF16)
    V_d = dscr("V_d", [8, 128, 64, 128], BF16)
    QT_d = dscr("QT_d", [8, 128, TOWN], BF16)
    uT_d = dscr("uT_d", [8, 128, TOWN], BF16)
    sga_d = dscr("sga_d", [16, 128, TOWN], BF16)
    sgb_d = dscr("sgb_d", [16, 128, TOWN], BF16)
    ysgu_d = dscr("ysgu_d", [8, 128, TOWN], BF16)
    yatt_d = dscr("yatt_d", [8, 128, TOWN], BF16)
    h1_d = dscr("h1_d", [TOWN, D], F32)
    hn2T_d = dscr("hn2T_d", [16, 128, TOWN], BF16)
    qpT_d = dscr("qpT_d", [16, 128, TOWN], BF16)
    G_d = dscr("G_d", [16, 128, 128, 128], BF16)
    h2_d = dscr("h2_d", [TOWN, D], F32)

    S = Sched(nc)
    d_KTd = S.deps(16, "KTd")
    d_Vd = S.deps(16, "Vd")
    d_QTd, d_uTd, d_sga, d_sgb, d_ysgu, d_yatt = (S.dep(n) for n in "QTd uTd sga sgb ysgu yatt".split())
    d_h1 = S.deps(16, "h1")
    d_hn2T, d_qpT = S.dep("hn2T"), S.dep("qpT")
    d_G = S.deps(16, "G")
    d_h2 = S.deps(16, "h2")
    d_out = S.dep("out")

    def bc(ap1, n):
        return ap1.to_broadcast([128, n])

    with S.phase() as P0:
        ident_f = P0.sb("ident_f", [128, 128], F32)
        ident_b = P0.sb("ident_b", [128, 128], BF16)
        kmT = P0.sb("kmT", [128, 8, 32], F32)
        kmT_b = P0.sb("kmT_b", [128, 8, 32], BF16)
        d_ident, d_kmT, d_kmTb = S.dep("ident"), S.dep("kmT"), S.dep("kmTb")
        S.dma("sp", ident_f[:], _ident_in.get(), writes=[d_ident], ch="ident")
        S.op("dve", lambda e: e.memset(kmT[:], 0.0), writes=[d_kmT])
        S.op("dve", lambda e: e.tensor_copy(out=ident_b[:], in_=ident_f[:]), reads=[d_ident], writes=[d_ident])

        def rms_norm(ph, src, d_src, gain, d_gain, dst, d_dst, tmp):
            st, d_st = tmp
            S.op("act", lambda e: e.activation(out=dst, in_=src, func=AF.Square, accum_out=st[:, 0:1]),
                 reads=[d_src], writes=[d_dst, d_st])
            S.op("dve", lambda e: e.tensor_scalar(out=st[:, 1:2], in0=st[:, 0:1], scalar1=1.0 / D, scalar2=EPS,
                                                  op0=ALU.mult, op1=ALU.add), reads=[d_st], writes=[d_st])
            S.op("act", lambda e: e.activation(out=st[:, 2:3], in_=st[:, 1:2], func=AF.Sqrt), reads=[d_st], writes=[d_st])
            S.op("dve", lambda e: e.reciprocal(out=st[:, 3:4], in_=st[:, 2:3]), reads=[d_st], writes=[d_st])
            S.op("dve", lambda e: e.scalar_tensor_tensor(out=dst, in0=src, scalar=st[:, 3:4], in1=gain,
                                                         op0=ALU.mult, op1=ALU.mult),
                 reads=[d_src, d_st, d_gain], writes=[d_dst])

        def norm_part1(xsrc, ntiles, gain, d_gain, xin, xn, stt):
            res = []
            for gt in range(ntiles):
                xt, d_xt, k = xin.nxt()
                S.dma("sp", xt[:], xsrc[gt * 128:(gt + 1) * 128, :], writes=[d_xt], ch=f"{id(xin)}_{k}")
                xnt, d_xn, _ = xn.nxt()
                stile, d_stile, _ = stt.nxt()
                rms_norm(None, xt[:], d_xt, gain, d_gain, xnt[:], d_xn, (stile, d_stile))
                res.append((xnt, d_xn))
            return res

        def trans_part2(xns, tp, dst_fn):
            for gt, (xnt, d_xn) in enumerate(xns):
                tpt, d_tp, _ = tp.nxt()
                for dc in range(16):
                    S.op("pe", lambda e, dc=dc, tpt=tpt, xnt=xnt: e.transpose(out=tpt[:, dc, :], in_=xnt[:, dc * 128:(dc + 1) * 128],
                                                                              identity=ident_b[:]),
                         reads=[d_xn, d_ident], writes=[d_tp])
                dst, d_dst = dst_fn(gt)
                S.op("act", lambda e, dst=dst, tpt=tpt: e.activation(out=dst, in_=tpt[:], func=AF.Copy), reads=[d_tp], writes=[d_dst])

        def norm_transpose(ph, xsrc, ntiles, gain, d_gain, xin, xn, tp, stt, dst_fn, src_deps=None):
            tiles = []
            for gt in range(ntiles):
                xt, d_xt, k = xin.nxt()
                tiles.append((xt, d_xt))
                S.dma("sp", xt[:], xsrc[gt * 128:(gt + 1) * 128, :], reads=([src_deps[gt]] if src_deps else []), writes=[d_xt],
                      ch=f"{id(xin)}_{k}")
                xnt, d_xn, _ = xn.nxt()
                stile, d_stile, _ = stt.nxt()
                rms_norm(ph, xt[:], d_xt, gain, d_gain, xnt[:], d_xn, (stile, d_stile))
                tpt, d_tp, _ = tp.nxt()
                for dc in range(16):
                    S.op("pe", lambda e, dc=dc, tpt=tpt, xnt=xnt: e.transpose(out=tpt[:, dc, :], in_=xnt[:, dc * 128:(dc + 1) * 128],
                                                                              identity=ident_b[:]),
                         reads=[d_xn, d_ident], writes=[d_tp])
                dst, d_dst = dst_fn(gt)
                S.op("act", lambda e, dst=dst, tpt=tpt: e.activation(out=dst, in_=tpt[:], func=AF.Copy), reads=[d_tp], writes=[d_dst])
            return tiles

        with (S.phase() if "TOPK" not in debug_outs else contextlib.nullcontext()) as ph:
          if ph is not None:
              wk = ph.sb("wk", [128, 16, 1024], BF16)
              wv = ph.sb("wv", [128, 16, 1024], BF16)
              gbc = ph.sb("gbc", [128, D], F32)
              d_wk, d_wv, d_g = S.deps(2, "wk"), S.deps(2, "wv"), S.dep("g")
              S.dma("sp", gbc[:], bc(_g_mix.get(), D), writes=[d_g], ch="gbc")
              for hf in range(2):
                  S.dma("pool", wk[:, :, hf * 512:(hf + 1) * 512],
                        _w_in.get()[:, C_K + hf * 512:C_K + (hf + 1) * 512].rearrange("(dc p) n -> p dc n", p=128), writes=[d_wk[hf]], ch=f"wk{hf}")
              for hf in range(2):
                  S.dma("pool", wv[:, :, hf * 512:(hf + 1) * 512],
                        _w_in.get()[:, C_V + hf * 512:C_V + (hf + 1) * 512].rearrange("(dc p) n -> p dc n", p=128), writes=[d_wv[hf]], ch=f"wv{hf}")
              xin = B(S, ph, "xin", [128, D], F32, 4)
              xn = B(S, ph, "xn", [128, D], BF16, 4)
              stt = B(S, ph, "stt", [128, 4], F32, 4)
              tp = B(S, ph, "tp", [128, 16, 128], BF16, 2, psum=True)
              xnT = B(S, ph, "xnT", [128, 16, 512], BF16, 2)
              KTs = B(S, ph, "KTs", [128, 8, 512], BF16, 2)
              Vs = B(S, ph, "Vs", [128, 4, 1024], BF16, 2)
              kp = B(S, ph, "kp", [128, 512], F32, 2, psum=True)
              vp = B(S, ph, "vp", [128, 512], F32, 2, psum=True)
              NSTA = int(os.environ.get("KDBG_NSTA", "16"))
              pre = norm_part1(_x_all.get()[0:512, :], 4, gbc[:], d_g, xin, xn, stt)
              for st in range(NSTA):
                  xT, d_xT, _ = xnT.nxt()
                  trans_part2(pre, tp, lambda gt, xT=xT, d_xT=d_xT: (xT[:, :, gt * 128:(gt + 1) * 128], d_xT))
                  if st + 1 < NSTA:
                      pre = norm_part1(_x_all.get()[(st + 1) * 512:(st + 2) * 512, :], 4, gbc[:], d_g, xin, xn, stt)
                  kt, d_kt, kti = KTs.nxt()
                  ADBG = int(os.environ.get("KDBG_A", "9"))
                  for h in range(8 if ADBG >= 2 else 0):
                      pk, d_pk, _ = kp.nxt()
                      for dc in range(16):
                          S.op("pe", lambda e, pk=pk, dc=dc, h=h, xT=xT: e.matmul(pk[:], lhsT=wk[:, dc, h * 128:(h + 1) * 128], rhs=xT[:, dc, :],
                                                                                  start=(dc == 0), stop=(dc == 15)),
                               reads=[d_wk[h // 4], d_xT], writes=[d_pk])
                      for bb in range(2):
                          S.op("act", lambda e, pk=pk, kt=kt, h=h, bb=bb, st=st: e.activation(
                              out=kt[:, h, bb * 256:(bb + 1) * 256], in_=pk[:, bb * 256:(bb + 1) * 256], func=AF.Copy,
                              accum_out=kmT[:, h, st * 2 + bb:st * 2 + bb + 1]), reads=[d_pk], writes=[d_kt, d_kmT])
                  vs, d_vs, vsi = Vs.nxt()
                  for tt in range(4 if ADBG >= 3 else 0):
                      for hf in range(2):
                          pv, d_pv, _ = vp.nxt()
                          for dc in range(16):
                              S.op("pe", lambda e, pv=pv, dc=dc, tt=tt, hf=hf, xT=xT: e.matmul(pv[:], lhsT=xT[:, dc, tt * 128:(tt + 1) * 128],
                                                                                             rhs=wv[:, dc, hf * 512:(hf + 1) * 512],
                                                                                             start=(dc == 0), stop=(dc == 15)),
                                   reads=[d_wv[hf], d_xT], writes=[d_pv])
                          S.op("dve", lambda e, pv=pv, vs=vs, tt=tt, hf=hf: e.tensor_copy(out=vs[:, tt, hf * 512:(hf + 1) * 512], in_=pv[:]),
                               reads=[d_pv], writes=[d_vs])
                  if ADBG >= 4:
                      S.dma("pool", KT_d[:, :, st * 512:(st + 1) * 512].rearrange("h p t -> p h t"), kt[:], reads=[d_kt], writes=[d_KTd[st]], ch=f"KTs{kti}")
                  for tt in range(4 if ADBG >= 5 else 0):
                      S.dma("pool", V_d[:, :, st * 4 + tt, :].rearrange("h p d -> p h d"), vs[:, tt, :].rearrange("p (h d) -> p h d", h=8),
                            reads=[d_vs], writes=[d_Vd[st]], ch=f"Vs{vsi}")
              S.op("dve", lambda e: e.tensor_scalar(out=kmT_b[:], in0=kmT[:], scalar1=1.0 / 256, scalar2=None, op0=ALU.mult),
                   reads=[d_kmT], writes=[d_kmTb])
        if "A" in debug_outs:
            S.close()
            return nc

        def mm(out_, lhsT, rhs, start, stop, reads, writes):
            S.op("pe", lambda e: e.matmul(out_, lhsT=lhsT, rhs=rhs, start=start, stop=stop), reads=reads, writes=writes)


        def proj_fm_g(wsrc, col0, ncols, func, xT, d_xT, dst, d_dst, wb, pp, stg):
            for cg in range(ncols // 512):
                w, d_w, wi = wb.nxt()
                S.dma("pool", w[:], wsrc[:, col0 + cg * 512:col0 + (cg + 1) * 512].rearrange("(dc p) n -> p dc n", p=128),
                      writes=[d_w], ch=f"{id(wb)}_{wi}")
                for fc in range(4):
                    sg, d_sg, si = stg.nxt()
                    for st in range(4):
                        pk, d_pk, _ = pp.nxt()
                        for dc in range(16):
                            mm(pk[:], w[:, dc, fc * 128:(fc + 1) * 128], xT[:, dc, st * 512:(st + 1) * 512], dc == 0, dc == 15,
                               [d_w, d_xT], [d_pk])
                        S.op("act", lambda e, sg=sg, pk=pk, st=st: e.activation(out=sg[:, st * 512:(st + 1) * 512], in_=pk[:], func=func),
                             reads=[d_pk], writes=[d_sg])
                    S.dma("sp", dst[cg * 4 + fc], sg[:], reads=[d_sg], writes=[d_dst], ch=f"{id(stg)}_{si}")


        class TopK:
            def __init__(self, ph, iota_c, d_iota):
                self.iota_c, self.d_iota = iota_c, d_iota
                mk = lambda n, sh, dt, k=1: B(S, ph, n, sh, dt, k)
                self.wk_ = mk("tk_wk", [128, 16, 128], F32)
                self.v16 = mk("tk_v16", [128, 16, 16], F32)
                self.ix16 = mk("tk_ix16", [128, 16, 16], U32)
                self.ixf = mk("tk_ixf", [128, 16, 16], F32)
                self.cand = mk("tk_cand", [128, 8, 256], F32)
                self.cand2 = mk("tk_cand2", [128, 8, 256], F32)
                self.c16 = mk("tk_c16", [128, 8, 16], F32)
                self.pos = mk("tk_pos", [128, 8, 16], U32)
                self.kab = mk("tk_kab", [128, 2, 8, 16], U32)
                self.kabf = mk("tk_kabf", [128, 2, 8, 16], F32)
                self.sm = mk("tk_sm", [128, 3, 8], F32)
                self.e16 = mk("tk_e16", [128, 8, 16], F32)
                self.eq = mk("tk_eq", [128, 8, 16, 16], F32)
                self.res = mk("tk_res", [128, 3, 128], F32)

            def run(self, sc, d_sc):
                nx = lambda b: b.nxt()[:2]
                wk_, d_wk_ = nx(self.wk_)
                v16, d_v = nx(self.v16)
                ix16, d_ix = nx(self.ix16)
                for hp in range(16):
                    S.op("dve", lambda e, hp=hp: e.max(out=v16[:, hp, 0:8], in_=sc[:, hp, :]), reads=[d_sc], writes=[d_v])
                for hp in range(16):
                    S.op("dve", lambda e, hp=hp: e.max_index(out=ix16[:, hp, 0:8], in_max=v16[:, hp, 0:8], in_values=sc[:, hp, :]),
                         reads=[d_sc, d_v], writes=[d_ix])
                for hp in range(16):
                    S.op("dve", lambda e, hp=hp: e.match_replace(out=wk_[:, hp, :], in_to_replace=v16[:, hp, 0:8], in_values=sc[:, hp, :],
                                                                 imm_value=-1e30), reads=[d_sc, d_v], writes=[d_wk_])
                for hp in range(16):
                    S.op("dve", lambda e, hp=hp: e.max(out=v16[:, hp, 8:16], in_=wk_[:, hp, :]), reads=[d_wk_], writes=[d_v])
                for hp in range(16):
                    S.op("dve", lambda e, hp=hp: e.max_index(out=ix16[:, hp, 8:16], in_max=v16[:, hp, 8:16], in_values=wk_[:, hp, :]),
                         reads=[d_wk_, d_v], writes=[d_ix])
                cand, d_c = nx(self.cand)
                cand2, d_c2 = nx(self.cand2)
                c16, d_c16 = nx(self.c16)
                pos, d_pos = nx(self.pos)
                for h in range(8):
                    S.op("dve", lambda e, h=h: e.tensor_tensor(out=cand[:, h, :].rearrange("p (a b) -> p a b", a=16),
                                                               in0=v16[:, 2 * h, :].unsqueeze(2).to_broadcast([128, 16, 16]),
                                                               in1=v16[:, 2 * h + 1, :].unsqueeze(1).to_broadcast([128, 16, 16]), op=ALU.add),
                         reads=[d_v], writes=[d_c])
                for h in range(8):
                    S.op("dve", lambda e, h=h: e.max(out=c16[:, h, 0:8], in_=cand[:, h, :]), reads=[d_c], writes=[d_c16])
                for h in range(8):
                    S.op("dve", lambda e, h=h: e.max_index(out=pos[:, h, 0:8], in_max=c16[:, h, 0:8], in_values=cand[:, h, :]),
                         reads=[d_c, d_c16], writes=[d_pos])
                for h in range(8):
                    S.op("dve", lambda e, h=h: e.match_replace(out=cand2[:, h, :], in_to_replace=c16[:, h, 0:8], in_values=cand[:, h, :],
                                                               imm_value=-1e30), reads=[d_c, d_c16], writes=[d_c2])
                for h in range(8):
                    S.op("dve", lambda e, h=h: e.max(out=c16[:, h, 8:16], in_=cand2[:, h, :]), reads=[d_c2], writes=[d_c16])
                for h in range(8):
                    S.op("dve", lambda e, h=h: e.max_index(out=pos[:, h, 8:16], in_max=c16[:, h, 8:16], in_values=cand2[:, h, :]),
                         reads=[d_c2, d_c16], writes=[d_pos])
                sm, d_sm = nx(self.sm)
                e16, d_e = nx(self.e16)
                res, d_res = nx(self.res)
                S.op("dve", lambda e: e.tensor_scalar(out=sm[:, 0, :], in0=c16[:, :, 0], scalar1=-1.0, scalar2=None, op0=ALU.mult),
                     reads=[d_c16], writes=[d_sm])
                for h in range(8):
                    S.op("act", lambda e, h=h: e.activation(out=e16[:, h, :], in_=c16[:, h, :], func=AF.Exp, bias=sm[:, 0, h:h + 1],
                                                            accum_out=sm[:, 1, h:h + 1]), reads=[d_c16, d_sm], writes=[d_e, d_sm])
                S.op("dve", lambda e: e.reciprocal(out=sm[:, 2, :], in_=sm[:, 1, :]), reads=[d_sm], writes=[d_sm])
                S.op("dve", lambda e: e.tensor_tensor(out=res[:, 2, :].rearrange("p (h k) -> p h k", h=8), in0=e16[:],
                                                      in1=sm[:, 2, :].unsqueeze(2).to_broadcast([128, 8, 16]), op=ALU.mult),
                     reads=[d_e, d_sm], writes=[d_res])
                kab, d_kab = nx(self.kab)
                kabf, d_kabf = nx(self.kabf)
                ixf, d_ixf = nx(self.ixf)
                S.op("dve", lambda e: e.tensor_scalar(out=kab[:, 0], in0=pos[:], scalar1=4, scalar2=None, op0=ALU.logical_shift_right),
                     reads=[d_pos], writes=[d_kab])
                S.op("dve", lambda e: e.tensor_scalar(out=kab[:, 1], in0=pos[:], scalar1=15, scalar2=None, op0=ALU.bitwise_and),
                     reads=[d_pos], writes=[d_kab])
                S.op("dve", lambda e: e.tensor_copy(out=kabf[:], in_=kab[:]), reads=[d_kab], writes=[d_kabf])
                S.op("dve", lambda e: e.tensor_copy(out=ixf[:], in_=ix16[:]), reads=[d_ix], writes=[d_ixf])
                eq, d_eq = nx(self.eq)
                iota16 = self.iota_c[:, 0:16].unsqueeze(1).unsqueeze(1).to_broadcast([128, 8, 16, 16])
                ixv = ixf[:].rearrange("p (h t) k -> p h t k", t=2)
                for t in range(2):
                    S.op("dve", lambda e, t=t: e.tensor_tensor(out=eq[:], in0=kabf[:, t].unsqueeze(3).to_broadcast([128, 8, 16, 16]),
                                                               in1=iota16, op=ALU.is_equal), reads=[d_kabf, self.d_iota], writes=[d_eq])
                    S.op("dve", lambda e, t=t: e.tensor_tensor(out=eq[:], in0=eq[:], in1=ixv[:, :, t, :].unsqueeze(2).to_broadcast([128, 8, 16, 16]),
                                                               op=ALU.mult), reads=[d_eq, d_ixf], writes=[d_eq])
                    S.op("dve", lambda e, t=t: e.tensor_reduce(out=res[:, t, :], in_=eq[:].rearrange("p h k a -> p (h k) a"), axis=AX.X, op=ALU.add),
                         reads=[d_eq], writes=[d_res])
                return res, d_res


        if "TOPK" in debug_outs:
            tk_sc = nc.dram_tensor("tk_sc", [128, 16, 128], F32, kind="ExternalInput").ap()
            tk_out = nc.dram_tensor("tk_out", [128, 3, 128], F32, kind="ExternalOutput").ap()
            with S.phase() as ph:
                iota_c = ph.sb("iota_c", [128, 128], F32)
                sct = ph.sb("sct", [128, 16, 128], F32)
                d_io, d_sct, d_o = S.dep(), S.dep(), S.dep()
                S.dma("sp", iota_c[:], bc(_iota_in.get(), 128), writes=[d_io], ch="iota")
                S.dma("sp", sct[:], tk_sc, writes=[d_sct], ch="sct")
                tk = TopK(ph, iota_c, d_io)
                res, d_res = tk.run(sct, d_sct)
                S.dma("sp", tk_out, res[:], reads=[d_res], writes=[d_o], ch="tko")
            S.close()
            return nc

        with S.phase() as phB:
            xnTo = phB.sb("xnTo", [128, 16, TOWN], BF16)
            d_xnTo = S.dep("xnTo")
            with S.phase() as ph:
                gbc = ph.sb("gbc", [128, D], F32)
                d_g = S.dep("g")
                S.dma("sp", gbc[:], bc(_g_mix.get(), D), writes=[d_g], ch="gbc")
                xin = B(S, ph, "xin", [128, D], F32, 2)
                xn = B(S, ph, "xn", [128, D], BF16, 2)
                stt = B(S, ph, "stt", [128, 4], F32, 2)
                tp = B(S, ph, "tp", [128, 16, 128], BF16, 2, psum=True)
                norm_transpose(ph, _x_own.get(), 16, gbc[:], d_g, xin, xn, tp, stt,
                               lambda gt: (xnTo[:, :, gt * 128:(gt + 1) * 128], d_xnTo))
            with S.phase() as ph:
                wb = B(S, ph, "wb", [128, 16, 512], BF16, 2)
                pp = B(S, ph, "pp", [128, 512], F32, 4, psum=True)
                stg = B(S, ph, "stg", [128, TOWN], BF16, 2)

                def proj_fm(col0, ncols, func, dst, d_dst):
                    proj_fm_g(_w_in.get(), col0, ncols, func, xnTo, d_xnTo, dst, d_dst, wb, pp, stg)

                proj_fm(C_Q, 1024, AF.Copy, QT_d, d_QTd)
                proj_fm(C_U, 1024, AF.Gelu, uT_d, d_uTd)
                proj_fm(C_GA, 2048, AF.Sigmoid, sga_d, d_sga)
                proj_fm(C_GB, 2048, AF.Sigmoid, sgb_d, d_sgb)
            with S.phase() as ph:
                wvg = ph.sb("wvg", [128, 16, 1024], BF16)
                lng = ph.sb("lng", [128, 1024], F32)
                lnb = ph.sb("lnb", [128, 1024], F32)
                bsb = ph.sb("bsb", [128, 1024], F32)
                msk = ph.sb("msk", [128, 128], F32)
                wnat = ph.sb("wnat", [128, 8, 128], F32)
                wsT = ph.sb("wsT", [128, 8, 128], BF16)
                d_ln, d_wsT = S.dep("b2c"), S.dep("wsT")
                d_bsb = d_msk = d_wnat = d_ln
                d_wvg = S.deps(2, "wvg")
                for hf in range(2):
                    S.dma("pool", wvg[:, :, hf * 512:(hf + 1) * 512],
                          _w_in.get()[:, C_VG + hf * 512:C_VG + (hf + 1) * 512].rearrange("(dc p) n -> p dc n", p=128), writes=[d_wvg[hf]], ch=f"wvg{hf}")
                S.dma("sp", lng[:], bc(_ln_g.get(), 1024), writes=[d_ln], ch="b2c")
                S.dma("sp", lnb[:], bc(_ln_b.get(), 1024), writes=[d_ln], ch="b2c")
                S.dma("sp", bsb[:], bc(_sgu_b.get(), 1024), writes=[d_bsb], ch="b2c")
                S.dma("sp", msk[:], _sgumask_in.get(), writes=[d_msk], ch="b2c")
                S.dma("sp", wnat[:], _sgu_w.get().rearrange("g t s -> t g s"), writes=[d_wnat], ch="b2c")
                mp = ph.ps("mp", [128, 8, 128], F32)
                d_mp = S.dep("mp")
                pv2 = B(S, ph, "pv2", [128, 512], F32, 2, psum=True)
                for g in range(8):
                    S.op("pe", lambda e, g=g: e.transpose(out=mp[:, g, :], in_=wnat[:, g, :], identity=ident_f[:]),
                         reads=[d_wnat, d_ident], writes=[d_mp])
                S.op("dve", lambda e: e.tensor_tensor(out=wsT[:], in0=mp[:], in1=msk[:].unsqueeze(1).to_broadcast([128, 8, 128]), op=ALU.mult),
                     reads=[d_mp, d_msk], writes=[d_wsT])
                vga = B(S, ph, "vga", [128, 1024], F32, 2)
                zz = B(S, ph, "zz", [128, 1024], F32, 2)
                vr = B(S, ph, "vr", [128, 1024], BF16, 2)
                ut = B(S, ph, "ut", [128, 8, 128], BF16, 2)
                ys = B(S, ph, "ys", [128, 8, 128], BF16, 2)
                sm = B(S, ph, "sm", [128, 24], F32, 2)
                for tt in range(16):
                    va, d_va, _ = vga.nxt()
                    for hf in range(2):
                        pk, d_pk, _ = pv2.nxt()
                        for dc in range(16):
                            mm(pk[:], xnTo[:, dc, tt * 128:(tt + 1) * 128], wvg[:, dc, hf * 512:(hf + 1) * 512], dc == 0, dc == 15,
                               [d_wvg[hf], d_xnTo], [d_pk])
                        S.op("act", lambda e, va=va, pk=pk, hf=hf: e.activation(out=va[:, hf * 512:(hf + 1) * 512], in_=pk[:], func=AF.Gelu),
                             reads=[d_pk], writes=[d_va])
                    m, d_m, _ = sm.nxt()
                    for hf in range(2):
                        S.op("dve", lambda e, m=m, va=va, hf=hf: e.bn_stats(out=m[:, hf * 6:(hf + 1) * 6], in_=va[:, hf * 512:(hf + 1) * 512]),
                             reads=[d_va], writes=[d_m])
                    S.op("dve", lambda e, m=m: e.bn_aggr(out=m[:, 12:14], in_=m[:, 0:12]), reads=[d_m], writes=[d_m])
                    S.op("dve", lambda e, m=m: e.tensor_scalar(out=m[:, 14:15], in0=m[:, 13:14], scalar1=EPS, scalar2=None, op0=ALU.add),
                         reads=[d_m], writes=[d_m])
                    S.op("act", lambda e, m=m: e.activation(out=m[:, 15:16], in_=m[:, 14:15], func=AF.Sqrt), reads=[d_m], writes=[d_m])
                    S.op("dve", lambda e, m=m: e.reciprocal(out=m[:, 16:17], in_=m[:, 15:16]), reads=[d_m], writes=[d_m])
                    z, d_z, _ = zz.nxt()
                    S.op("dve", lambda e, z=z, va=va, m=m: e.tensor_scalar(out=z[:], in0=va[:], scalar1=m[:, 12:13], scalar2=m[:, 16:17],
                                                                          op0=ALU.subtract, op1=ALU.mult), reads=[d_va, d_m], writes=[d_z])
                    S.op("dve", lambda e, z=z: e.tensor_tensor(out=z[:], in0=z[:], in1=lng[:], op=ALU.mult), reads=[d_z, d_ln], writes=[d_z])
                    v, d_v, _ = vr.nxt()
                    S.op("dve", lambda e, z=z, v=v: e.tensor_tensor(out=v[:], in0=z[:], in1=lnb[:], op=ALU.add), reads=[d_z, d_ln], writes=[d_v])
                    for g in range(8):
                        mm(mp[:, g, :], v[:, g * 128:(g + 1) * 128], wsT[:, g, :], True, True, [d_v, d_wsT], [d_mp])
                    u, d_u, ui = ut.nxt()
                    S.dma("sp", u[:], uT_d[:, :, tt * 128:(tt + 1) * 128].rearrange("g p t -> p g t"), reads=[d_uTd], writes=[d_u], ch=f"ut{ui}")
                    S.op("dve", lambda e, z=z: e.tensor_tensor(out=z[:].rearrange("p (g t) -> p g t", g=8), in0=mp[:],
                                                                in1=bsb[:].rearrange("p (g t) -> p g t", g=8), op=ALU.add),
                         reads=[d_mp, d_bsb, d_v], writes=[d_z])
                    y, d_y, yi = ys.nxt()
                    S.op("dve", lambda e, z=z, y=y, u=u: e.tensor_tensor(out=y[:], in0=z[:].rearrange("p (g t) -> p g t", g=8), in1=u[:], op=ALU.mult),
                         reads=[d_z, d_u], writes=[d_y])
                    S.dma("pool", ysgu_d[:, :, tt * 128:(tt + 1) * 128].rearrange("g p t -> p g t"), y[:], reads=[d_y], writes=[d_ysgu], ch=f"ys{yi}")
        if "B" in debug_outs:
            S.close()
            return nc

        with S.phase() as ph:
            pastb = ph.sb("pastb", [128, 256], F32)
            ownm = ph.sb("ownm", [128, 256], F32)
            d_pm = S.dep("pm")
            S.dma("sp", pastb[:], bc(_pastb_in.get(), 256), writes=[d_pm], ch="pastb")
            S.dma("sp", ownm[:], bc(_ownm_in.get(), 256), writes=[d_pm], ch="pastb")
            KTb = B(S, ph, "KTb", [128, TALL], BF16, 2)
            Vb = B(S, ph, "Vb", [128, 64, 130], BF16, 2)
            for k_ in range(2):
                S.op("pool", lambda e, k_=k_: e.memset(Vb.t[k_][:, :, 128:130], 1.0), writes=[Vb.d[k_]])
            Qb = B(S, ph, "Qb", [128, 256], BF16, 2)
            cbb = B(S, ph, "cbb", [128, 4, 2, 256], BF16, 2)
            spb = B(S, ph, "spb", [128, 512], F32, 3, psum=True)
            ptp = B(S, ph, "ptp", [128, 8, 128], BF16, 2, psum=True)
            opb = B(S, ph, "opb", [128, 512], F32, 2, psum=True)
            gp = ph.ps("gp", [128, 64], F32)
            d_gp = S.dep("gp")
            otp = ptp.t[0][:, 7, :]
            d_otp = ptp.d[0]
            gmb = B(S, ph, "gmb", [128, 32], F32, 2)
            t8b = B(S, ph, "t8b", [128, 10], F32, 2)
            ebb = B(S, ph, "ebb", [128, 32], F32, 2)
            rsb = B(S, ph, "rsb", [128, 2], F32, 2)
            Pb = B(S, ph, "Pb", [128, 512], BF16, 4)
            PTb = B(S, ph, "PTb", [128, 4, 128], BF16, 3)
            Onb = B(S, ph, "Onb", [128, 128], BF16, 2)
            ystb = B(S, ph, "ystb", [128, 256], BF16, 2)
            items = [(i, h) for i in range(int(os.environ.get("KDBG_NI", "8"))) for h in range(8)]
            loaded = {}
            cbl = {}

            def load(idx):
                i, h = items[idx]
                nblk = 4 * i + 4
                if i not in cbl:
                    c_, d_c, ci = cbb.nxt()
                    S.dma("sp", c_[:].rearrange("p a b k -> p (a b k)"), _cb_in.get()[i], writes=[d_c], ch=f"cbb{ci}")
                    cbl[i] = (c_, d_c)
                kt, d_kt, ki = KTb.nxt()
                S.dma("sp", kt[:, 0:nblk * 256], KT_d[h][:, 0:nblk * 256], reads=d_KTd[:nblk // 2], writes=[d_kt], ch=f"KTb{ki}")
                vt, d_vt, vi = Vb.nxt()
                S.dma("sp", vt[:, 0:nblk * 2, 0:128], V_d[h][:, 0:nblk * 2, :], reads=d_Vd[:nblk // 2], writes=[d_vt], ch=f"Vb{vi}")
                qt, d_qt, qi = Qb.nxt()
                S.dma("sp", qt[:], QT_d[h][:, i * 256:(i + 1) * 256], reads=[d_QTd], writes=[d_qt], ch=f"Qb{qi}")
                loaded[idx] = (kt, d_kt, vt, d_vt, qt, d_qt)

            load(0)
            for idx, (i, h) in enumerate(items):
                if idx + 1 < len(items):
                    load(idx + 1)
                kt, d_kt, vt, d_vt, qt, d_qt = loaded.pop(idx)
                cbt, d_cb = cbl[i]
                nblk = 4 * i + 4
                nbp = nblk // 2
                yst, d_yst, ysi = ystb.nxt()
                ebs = []
                for hq in range(2):
                    q = qt[:, hq * 128:(hq + 1) * 128]
                    mm(gp[:, hq * 32:(hq + 1) * 32], q, kmT_b[:, h, :], True, True, [d_qt, d_kmTb], [d_gp])
                    gm, d_gm, _ = gmb.nxt()
                    S.op("dve", lambda e, gm=gm, i=i, hq=hq: e.tensor_tensor(out=gm[:], in0=gp[:, hq * 32:(hq + 1) * 32], in1=pastb[:, i * 32:(i + 1) * 32], op=ALU.add),
                         reads=[d_gp, d_pm], writes=[d_gm])
                    t8, d_t8, _ = t8b.nxt()
                    S.op("dve", lambda e, gm=gm, t8=t8: e.max(out=t8[:, 0:8], in_=gm[:]), reads=[d_gm], writes=[d_t8])
                    S.op("dve", lambda e, t8=t8: e.tensor_scalar(out=t8[:, 8:9], in0=t8[:, 2:3], scalar1=-1e29, scalar2=None, op0=ALU.max),
                         reads=[d_t8], writes=[d_t8])
                    eb, d_eb, _ = ebb.nxt()
                    S.op("dve", lambda e, eb=eb, gm=gm, t8=t8: e.tensor_scalar(out=eb[:], in0=gm[:], scalar1=t8[:, 8:9], scalar2=None, op0=ALU.is_ge),
                         reads=[d_gm, d_t8], writes=[d_eb])
                    S.op("dve", lambda e, eb=eb, i=i: e.tensor_tensor(out=eb[:], in0=eb[:], in1=ownm[:, i * 32:(i + 1) * 32], op=ALU.max),
                         reads=[d_eb, d_pm], writes=[d_eb])
                    S.op("dve", lambda e, eb=eb: e.tensor_scalar(out=eb[:], in0=eb[:], scalar1=-1.0, scalar2=-NEGB, op0=ALU.add, op1=ALU.mult),
                         reads=[d_eb], writes=[d_eb])
                    ebs.append((eb, d_eb))
                for hq in range(2):
                    q = qt[:, hq * 128:(hq + 1) * 128]
                    eb, d_eb = ebs[hq]
                    rs, d_rs, _ = rsb.nxt()
                    o_ps, d_o, _ = opb.nxt()
                    hs = {}

                    def st12(bp):
                        s_t, d_s, _ = spb.nxt()
                        if 2 * bp + 1 < 4 * i:
                            mm(s_t[:], q, kt[:, bp * 512:(bp + 1) * 512], True, True, [d_qt, d_kt], [d_s])
                        else:
                            for nb in range(2):
                                n = 2 * bp + nb
                                causal = n >= 4 * i
                                mm(s_t[:, nb * 256:(nb + 1) * 256], q, kt[:, n * 256:(n + 1) * 256], True, not causal, [d_qt, d_kt], [d_s])
                                if causal:
                                    mm(s_t[:, nb * 256:(nb + 1) * 256], ident_b[:], cbt[:, n - 4 * i, hq, :], False, True, [d_ident, d_cb], [d_s])
                        P, d_P, _ = Pb.nxt()
                        for nb in range(2):
                            n = 2 * bp + nb
                            S.op("act", lambda e, P=P, s_t=s_t, nb=nb, n=n, eb=eb: e.activation(
                                out=P[:, nb * 256:(nb + 1) * 256], in_=s_t[:, nb * 256:(nb + 1) * 256], func=AF.Exp, scale=SCALE,
                                bias=eb[:, n:n + 1]), reads=[d_s, d_eb], writes=[d_P])
                        hs[bp] = [P, d_P]

                    def st34(bp):
                        P, d_P = hs[bp]
                        pp_, d_pp, _ = ptp.nxt()
                        for kk in range(4):
                            S.op("pe", lambda e, pp_=pp_, P=P, kk=kk: e.transpose(out=pp_[:, kk, :], in_=P[:, kk * 128:(kk + 1) * 128], identity=ident_b[:]),
                                 reads=[d_P, d_ident], writes=[d_pp])
                        PT, d_PT, _ = PTb.nxt()
                        S.op("dve", lambda e, PT=PT, pp_=pp_: e.tensor_copy(out=PT[:], in_=pp_[:, 0:4, :]), reads=[d_pp], writes=[d_PT])
                        hs[bp] = [PT, d_PT]

                    def st5(bp):
                        PT, d_PT = hs.pop(bp)
                        for kk in range(4):
                            mm(o_ps[:, 0:130], PT[:, kk, :], vt[:, bp * 4 + kk, :], bp == 0 and kk == 0, bp == nbp - 1 and kk == 3,
                               [d_PT, d_vt], [d_o])

                    for step in range(nbp + 3):
                        if step < nbp:
                            st12(step)
                        if 0 <= step - 2 < nbp:
                            st34(step - 2)
                        if 0 <= step - 3 < nbp:
                            st5(step - 3)
                    S.op("dve", lambda e, rs=rs, o_ps=o_ps: e.reciprocal(out=rs[:, 0:1], in_=o_ps[:, 128:129]), reads=[d_o], writes=[d_rs])
                    On, d_On, _ = Onb.nxt()
                    S.op("act", lambda e, On=On, o_ps=o_ps, rs=rs: e.activation(out=On[:], in_=o_ps[:, 0:128], func=AF.Copy, scale=rs[:, 0:1]),
                         reads=[d_o, d_rs], writes=[d_On])
                    S.op("pe", lambda e, On=On: e.transpose(out=otp, in_=On[:], identity=ident_b[:]), reads=[d_On, d_ident], writes=[d_otp])
                    S.op("dve", lambda e, yst=yst, hq=hq: e.tensor_copy(out=yst[:, hq * 128:(hq + 1) * 128], in_=otp),
                         reads=[d_otp], writes=[d_yst])
                S.dma("pool", yatt_d[h][:, i * 256:(i + 1) * 256], yst[:], reads=[d_yst], writes=[d_yatt], ch=f"yst{ysi}")
        if "C" in debug_outs:
            S.close()
            return nc

        with S.phase() as phD:
            mergedT = phD.sb("mergedT", [128, 16, TOWN], BF16)
            d_mg = S.dep("mergedT")
            with S.phase() as ph:
                yat = ph.sb("yat", [128, 8, TOWN], BF16)
                ysg = ph.sb("ysg", [128, 8, TOWN], BF16)
                d_yat, d_ysg = S.dep("yat"), S.dep("ysg")
                S.dma("sp", yat[:], yatt_d.rearrange("a p t -> p a t"), reads=[d_yatt], writes=[d_yat], ch="yat")
                S.dma("sp", ysg[:], ysgu_d.rearrange("a p t -> p a t"), reads=[d_ysgu], writes=[d_ysg], ch="ysg")
                wab = B(S, ph, "wab", [128, 8, 512], BF16, 2)
                wsb = B(S, ph, "wsb", [128, 8, 512], BF16, 2)
                gab = B(S, ph, "gab", [128, TOWN], BF16, 2)
                gbb = B(S, ph, "gbb", [128, TOWN], BF16, 2)
                pa = B(S, ph, "pa", [128, 512], F32, 2, psum=True)
                pb = B(S, ph, "pb", [128, 512], F32, 2, psum=True)
                t1b = B(S, ph, "t1b", [128, 512], F32, 2)
                t2b = B(S, ph, "t2b", [128, 512], F32, 2)
                for cg in range(4):
                    wa, d_wa, wai = wab.nxt()
                    S.dma("pool", wa[:], _w_ba.get()[:, cg * 512:(cg + 1) * 512].rearrange("(a p) n -> p a n", p=128), writes=[d_wa], ch=f"wab{wai}")
                    ws, d_ws, wsi = wsb.nxt()
                    S.dma("pool", ws[:], _w_bs.get()[:, cg * 512:(cg + 1) * 512].rearrange("(a p) n -> p a n", p=128), writes=[d_ws], ch=f"wsb{wsi}")
                    for fc4 in range(4):
                        fc = cg * 4 + fc4
                        ga, d_ga, gai = gab.nxt()
                        S.dma("sp", ga[:], sga_d[fc], reads=[d_sga], writes=[d_ga], ch=f"gab{gai}")
                        gb, d_gb, gbi = gbb.nxt()
                        S.dma("sp", gb[:], sgb_d[fc], reads=[d_sgb], writes=[d_gb], ch=f"gbb{gbi}")
                        for st in range(4):
                            A, d_A, _ = pa.nxt()
                            for a_ in range(8):
                                mm(A[:], wa[:, a_, fc4 * 128:(fc4 + 1) * 128], yat[:, a_, st * 512:(st + 1) * 512], a_ == 0, a_ == 7, [d_wa, d_yat], [d_A])
                            Sg, d_Sg, _ = pb.nxt()
                            for a_ in range(8):
                                mm(Sg[:], ws[:, a_, fc4 * 128:(fc4 + 1) * 128], ysg[:, a_, st * 512:(st + 1) * 512], a_ == 0, a_ == 7, [d_ws, d_ysg], [d_Sg])
                            T1, d_T1, _ = t1b.nxt()
                            S.op("dve", lambda e, T1=T1, A=A, ga=ga, st=st: e.tensor_tensor(out=T1[:], in0=A[:], in1=ga[:, st * 512:(st + 1) * 512], op=ALU.mult),
                                 reads=[d_A, d_ga], writes=[d_T1])
                            T2, d_T2, _ = t2b.nxt()
                            S.op("dve", lambda e, T2=T2, Sg=Sg, gb=gb, st=st: e.tensor_tensor(out=T2[:], in0=Sg[:], in1=gb[:, st * 512:(st + 1) * 512], op=ALU.mult),
                                 reads=[d_Sg, d_gb], writes=[d_T2])
                            S.op("pool", lambda e, T1=T1, T2=T2, fc=fc, st=st: e.tensor_tensor(out=mergedT[:, fc, st * 512:(st + 1) * 512], in0=T1[:], in1=T2[:], op=ALU.add),
                                 reads=[d_T1, d_T2], writes=[d_mg])
            with S.phase() as ph:
                wo = B(S, ph, "wo", [128, 16, 512], BF16, 2)
                xr = B(S, ph, "xr", [128, 512], F32, 3)
                ho = B(S, ph, "ho", [128, 512], F32, 3)
                po = B(S, ph, "po", [128, 512], F32, 2, psum=True)
                for cg in range(4):
                    w, d_w, wi = wo.nxt()
                    S.dma("pool", w[:], _w_out.get()[:, cg * 512:(cg + 1) * 512].rearrange("(dc p) n -> p dc n", p=128), writes=[d_w], ch=f"wo{wi}")
                    for tt in range(16):
                        x_, d_x, xi = xr.nxt()
                        S.dma("sp", x_[:], _x_own.get()[tt * 128:(tt + 1) * 128, cg * 512:(cg + 1) * 512], writes=[d_x], ch=f"xr{xi}")
                        P, d_P, _ = po.nxt()
                        for fc in range(16):
                            mm(P[:], mergedT[:, fc, tt * 128:(tt + 1) * 128], w[:, fc, :], fc == 0, fc == 15, [d_mg, d_w], [d_P])
                        h_, d_h, hi = ho.nxt()
                        S.op("dve", lambda e, h_=h_, P=P, x_=x_: e.tensor_tensor(out=h_[:], in0=P[:], in1=x_[:], op=ALU.add), reads=[d_P, d_x], writes=[d_h])
                        S.dma("pool", h1_d[tt * 128:(tt + 1) * 128, cg * 512:(cg + 1) * 512], h_[:], reads=[d_h], writes=[d_h1[tt]], ch=f"ho{hi}")
        if "D" in debug_outs:
            S.close()
            return nc

        with S.phase() as phE:
            hn2T = phE.sb("hn2T", [128, 16, TOWN], BF16)
            d_hn = S.dep("hn2T_sb")
            with S.phase() as ph:
                gbc = ph.sb("gbc", [128, D], F32)
                d_g = S.dep("g")
                S.dma("sp", gbc[:], bc(_g_ffn.get(), D), writes=[d_g], ch="gbc2")
                xin = B(S, ph, "xin", [128, D], F32, 2)
                xn = B(S, ph, "xn", [128, D], BF16, 2)
                stt = B(S, ph, "stt", [128, 4], F32, 2)
                tp = B(S, ph, "tp", [128, 16, 128], BF16, 2, psum=True)
                norm_transpose(ph, h1_d, 16, gbc[:], d_g, xin, xn, tp, stt,
                               lambda gt: (hn2T[:, :, gt * 128:(gt + 1) * 128], d_hn), src_deps=d_h1)
                S.dma("sp", hn2T_d.rearrange("c p t -> p c t"), hn2T[:], reads=[d_hn], writes=[d_hn2T], ch="hn2Tst")
            with S.phase() as ph:
                wb = B(S, ph, "wb", [128, 16, 512], BF16, 2)
                pp = B(S, ph, "pp", [128, 512], F32, 4, psum=True)
                stg = B(S, ph, "stg", [128, TOWN], BF16, 2)
                proj_fm_g(_w_pq.get(), 0, 2048, AF.Copy, hn2T, d_hn, qpT_d, d_qpT, wb, pp, stg)
        with S.phase() as ph:
            iota_c = ph.sb("iota_c", [128, 128], F32)
            sknat = ph.sb("sknat", [128, 16, 128], F32)
            skT = ph.sb("skT", [128, 16, 128], BF16)
            d_io, d_sknat, d_skT = S.dep("iota"), S.dep("sknat"), S.dep("skT")
            S.dma("sp", iota_c[:], bc(_iota_in.get(), 128), writes=[d_io], ch="iota")
            S.dma("sp", sknat[:], _subk.get().rearrange("c n k -> n c k"), writes=[d_sknat], ch="sknat")
            scp = ph.ps("scp", [128, 16, 128], F32)
            d_scp = S.dep("scp")
            for hp in range(16):
                S.op("pe", lambda e, hp=hp: e.transpose(out=scp[:, hp, :], in_=sknat[:, hp, :], identity=ident_f[:]), reads=[d_sknat, d_ident], writes=[d_scp])
            S.op("dve", lambda e: e.tensor_copy(out=skT[:], in_=scp[:]), reads=[d_scp], writes=[d_skT])
            tk = TopK(ph, iota_c, d_io)
            qpb = B(S, ph, "qpb", [128, 16, 128], BF16, 3)
            scb = B(S, ph, "scb", [128, 16, 128], F32, 3)
            tr3 = ph.ps("tr3", [128, 4, 128], F32)
            d_tr3 = S.dep("tr3")
            T3b = B(S, ph, "T3b", [128, 3, 128], BF16, 2)
            iota_b = ph.sb("iota_b", [128, 128], BF16)
            S.op("dve", lambda e: e.tensor_copy(out=iota_b[:], in_=iota_c[:]), reads=[d_io], writes=[d_io])
            Pall = B(S, ph, "Pall", [128, 64, 128], BF16, 2)
            Qall = B(S, ph, "Qall", [128, 64, 128], BF16, 2)
            gtp = B(S, ph, "gtp", [128, 4, 128], F32, 2, psum=True)
            GTs = B(S, ph, "GTs", [128, 128, 128], BF16, 1)
            iob = iota_b[:].unsqueeze(1).to_broadcast([128, 64, 128])
            NTE = int(os.environ.get("KDBG_NTE", "16"))

            def e_scores(tt):
                qp, d_qp, qi = qpb.nxt()
                S.dma("sp", qp[:], qpT_d[:, :, tt * 128:(tt + 1) * 128].rearrange("c p t -> p c t"), reads=[d_qpT], writes=[d_qp], ch=f"qpb{qi}")
                for hp in range(16):
                    mm(scp[:, hp, :], qp[:, hp, :], skT[:, hp, :], True, True, [d_qp, d_skT], [d_scp])
                sc, d_sc, _ = scb.nxt()
                S.op("act", lambda e, sc=sc: e.activation(out=sc[:], in_=scp[:], func=AF.Copy), reads=[d_scp], writes=[d_sc])
                return sc, d_sc

            def e_topk(sc, d_sc):
                res, d_res = tk.run(sc, d_sc)
                for t in range(3):
                    S.op("pe", lambda e, t=t, res=res: e.transpose(out=tr3[:, t, :], in_=res[:, t, :], identity=ident_f[:]), reads=[d_res, d_ident], writes=[d_tr3])
                T3, d_T3, _ = T3b.nxt()
                S.op("dve", lambda e, T3=T3: e.tensor_copy(out=T3[:], in_=tr3[:, 0:3, :]), reads=[d_tr3], writes=[d_T3])
                return T3, d_T3

            def e_build(tt, T3, d_T3):
                gts, d_gts, gi = GTs.nxt()
                for half in range(2):
                    P_, d_P, _ = Pall.nxt()
                    Q_, d_Q, _ = Qall.nxt()
                    lo = half * 64
                    S.op("dve", lambda e, P_=P_, T3=T3, lo=lo: e.tensor_tensor(out=P_[:], in0=iob, in1=T3[:, 0, lo:lo + 64].unsqueeze(2).to_broadcast([128, 64, 128]),
                                                                           op=ALU.is_equal), reads=[d_T3, d_io], writes=[d_P])
                    S.op("dve", lambda e, P_=P_, T3=T3, lo=lo: e.tensor_tensor(out=P_[:], in0=P_[:], in1=T3[:, 2, lo:lo + 64].unsqueeze(2).to_broadcast([128, 64, 128]),
                                                                           op=ALU.mult), reads=[d_T3, d_P], writes=[d_P])
                    S.op("dve", lambda e, Q_=Q_, T3=T3, lo=lo: e.tensor_tensor(out=Q_[:], in0=iob, in1=T3[:, 1, lo:lo + 64].unsqueeze(2).to_broadcast([128, 64, 128]),
                                                                           op=ALU.is_equal), reads=[d_T3, d_io], writes=[d_Q])
                    for t4 in range(16):
                        g_, d_g_, _ = gtp.nxt()
                        for u in range(4):
                            tl = t4 * 4 + u
                            mm(g_[:, u, :], Q_[:, tl, :], P_[:, tl, :], True, True, [d_Q, d_P], [d_g_])
                        c0 = lo + t4 * 4
                        S.op("act", lambda e, g_=g_, gts=gts, c0=c0: e.activation(out=gts[:, :, c0:c0 + 4], in_=g_[:].rearrange("p t i -> p i t"), func=AF.Copy),
                             reads=[d_g_], writes=[d_gts])
                S.dma("pool", G_d[tt], gts[:], reads=[d_gts], writes=[d_G[tt]], ch=f"GTs{gi}")

            scs = [e_scores(0)]
            if NTE > 1:
                scs.append(e_scores(1))
            t3s = [e_topk(*scs.pop(0))]
            for tt in range(NTE):
                if tt + 2 < NTE:
                    scs.append(e_scores(tt + 2))
                if tt + 1 < NTE:
                    t3s.append(e_topk(*scs.pop(0)))
                e_build(tt, *t3s.pop(0))
        if "E" in debug_outs:
            S.close()
            return nc

        with S.phase() as ph:
            hnb = B(S, ph, "hnb", [128, 16, 1024], BF16, 1)
            yacc = ph.sb("yacc", [128, 8, D], F32)
            d_yacc = S.deps(8, "yacc")
            dnb = B(S, ph, "dnb", [128, 4, D], BF16, 2)
            upb = B(S, ph, "upb", [128, 4, D], BF16, 2)
            dTb = B(S, ph, "dTb", [128, 4, 16, 128], BF16, 2)
            gtb = B(S, ph, "gtb", [128, 4, 128], BF16, 3)
            geb = B(S, ph, "geb", [128, 4, 128], BF16, 2)
            wtb = B(S, ph, "wtb", [128, 4, 128], BF16, 2)
            tpF = B(S, ph, "tpF", [128, 16, 128], BF16, 1, psum=True)
            hpF = B(S, ph, "hpF", [128, 4, 128], F32, 2, psum=True)
            ypF = B(S, ph, "ypF", [128, 512], F32, 4, psum=True)
            h1t = B(S, ph, "h1t", [128, 1024], F32, 1)
            NSPAN = int(os.environ.get("KDBG_NSPAN", "32"))

            def f_load(s_):
                dn, d_dn, dni = dnb.nxt()
                S.dma("pool", dn[:], _p_down.get()[s_ * 512:(s_ + 1) * 512, :].rearrange("(ic p) d -> p ic d", p=128), writes=[d_dn], ch=f"dnb{dni}")
                up, d_up, upi = upb.nxt()
                S.dma("pool", up[:], _p_up.get()[s_ * 512:(s_ + 1) * 512, :].rearrange("(ic p) d -> p ic d", p=128), writes=[d_up], ch=f"upb{upi}")
                return dn, d_dn, up, d_up

            def f_trans(dn, d_dn):
                dT, d_dT, _ = dTb.nxt()
                for ic in range(4):
                    tpp, d_tpp, _ = tpF.nxt()
                    for dc in range(16):
                        S.op("pe", lambda e, tpp=tpp, dn=dn, ic=ic, dc=dc: e.transpose(out=tpp[:, dc, :], in_=dn[:, ic, dc * 128:(dc + 1) * 128], identity=ident_b[:]),
                             reads=[d_dn, d_ident], writes=[d_tpp])
                    S.op("act", lambda e, tpp=tpp, dT=dT, ic=ic: e.activation(out=dT[:, ic], in_=tpp[:], func=AF.Copy), reads=[d_tpp], writes=[d_dT])
                return dT, d_dT

            def f_hid(g, s_, tt, dT, d_dT, hnt, d_hnt):
                T = g * 8 + tt
                gt, d_gt, gti = gtb.nxt()
                S.dma("sp", gt[:], G_d[T][:, s_ * 4:(s_ + 1) * 4, :], reads=[d_G[T]], writes=[d_gt], ch=f"gtb{gti}")
                hp_, d_hp, _ = hpF.nxt()
                for ic in range(4):
                    for dc in range(16):
                        mm(hp_[:, ic, :], dT[:, ic, dc, :], hnt[:, dc, tt * 128:(tt + 1) * 128], dc == 0, dc == 15, [d_dT, d_hnt], [d_hp])
                ge, d_ge, _ = geb.nxt()
                S.op("act", lambda e, ge=ge, hp_=hp_: e.activation(out=ge[:], in_=hp_[:], func=AF.Gelu), reads=[d_hp], writes=[d_ge])
                wt, d_wt, _ = wtb.nxt()
                S.op("dve", lambda e, wt=wt, ge=ge, gt=gt: e.tensor_tensor(out=wt[:], in0=ge[:], in1=gt[:], op=ALU.mult), reads=[d_ge, d_gt], writes=[d_wt])
                return wt, d_wt

            def f_up(s_, tt, wt, d_wt, up, d_up):
                for c in range(4):
                    yp, d_yp, _ = ypF.nxt()
                    for ic in range(4):
                        mm(yp[:], wt[:, ic, :], up[:, ic, c * 512:(c + 1) * 512], ic == 0, ic == 3, [d_wt, d_up], [d_yp])
                    if s_ == 0:
                        S.op("dve", lambda e, yp=yp, tt=tt, c=c: e.tensor_copy(out=yacc[:, tt, c * 512:(c + 1) * 512], in_=yp[:]),
                             reads=[d_yp], writes=[d_yacc[tt]])
                    else:
                        S.op("dve", lambda e, yp=yp, tt=tt, c=c: e.tensor_tensor(out=yacc[:, tt, c * 512:(c + 1) * 512], in0=yp[:],
                                                                                 in1=yacc[:, tt, c * 512:(c + 1) * 512], op=ALU.add),
                             reads=[d_yp, d_yacc[tt]], writes=[d_yacc[tt]])

            for g in range(2):
                hnt, d_hnt, _ = hnb.nxt()
                S.dma("sp", hnt[:], hn2T_d[:, :, g * 1024:(g + 1) * 1024].rearrange("c p t -> p c t"), reads=[d_hn2T], writes=[d_hnt], ch="hnb")
                ld = f_load(0)
                nxt_ld = f_load(1) if NSPAN > 1 else None
                cur_T = f_trans(ld[0], ld[1])
                pend = None
                for s_ in range(NSPAN):
                    dT, d_dT = cur_T
                    up, d_up = ld[2], ld[3]
                    for tt in range(8):
                        wt, d_wt = f_hid(g, s_, tt, dT, d_dT, hnt, d_hnt)
                        if pend is not None:
                            f_up(*pend)
                        pend = (s_, tt, wt, d_wt, up, d_up)
                        if tt == 0 and s_ >= 1 and s_ + 1 < NSPAN:
                            nxt_ld = f_load(s_ + 1)
                        if tt == 3 and s_ + 1 < NSPAN:
                            nxt_T = f_trans(nxt_ld[0], nxt_ld[1])
                    if s_ + 1 < NSPAN:
                        ld = nxt_ld
                        cur_T = nxt_T
                f_up(*pend)
                for tt in range(8):
                    T = g * 8 + tt
                    for hf in range(2):
                        cs = slice(hf * 1024, (hf + 1) * 1024)
                        h1, d_h1t, _ = h1t.nxt()
                        S.dma("sp", h1[:], h1_d[T * 128:(T + 1) * 128, cs], reads=[d_h1[T]], writes=[d_h1t], ch="h1t")
                        S.op("pool", lambda e, h1=h1, tt=tt, cs=cs: e.tensor_tensor(out=h1[:], in0=h1[:], in1=yacc[:, tt, cs], op=ALU.add),
                             reads=[d_h1t, d_yacc[tt]], writes=[d_h1t])
                        S.dma("sp", h2_d[T * 128:(T + 1) * 128, cs], h1[:], reads=[d_h1t], writes=[d_h2[T]], ch="h1t_out")
        if "F" in debug_outs:
            S.close()
            return nc

        with S.phase() as ph:
            wpg = ph.sb("wpg", [128, 16, D], BF16)
            wpp = ph.sb("wpp", [128, 2, D], BF16)
            gple = ph.sb("gple", [128, D], F32)
            gfin = ph.sb("gfin", [128, D], F32)
            d_wpp, d_gple = S.dep("wpp"), S.dep("gpf")
            d_gfin = d_gple
            d_wpg = S.deps(4, "wpg")
            for cg in range(4):
                S.dma("pool", wpg[:, :, cg * 512:(cg + 1) * 512], _w_pg.get()[:, cg * 512:(cg + 1) * 512].rearrange("(dc p) n -> p dc n", p=128),
                      writes=[d_wpg[cg]], ch=f"wpg{cg}")
            S.dma("pool", wpp[:], _w_pp.get().rearrange("(c p) n -> p c n", p=128), writes=[d_wpp], ch="wpp")
            S.dma("sp", gple[:], bc(_g_ple.get(), D), writes=[d_gple], ch="gple")
            S.dma("sp", gfin[:], bc(_g_fin.get(), D), writes=[d_gfin], ch="gple")
            xin = B(S, ph, "xin", [128, D], F32, 4)
            xn = B(S, ph, "xn", [128, D], BF16, 2)
            stt = B(S, ph, "stt", [128, 4], F32, 3)
            stt2 = B(S, ph, "stt2", [128, 4], F32, 2)
            tp = B(S, ph, "tp", [128, 16, 128], BF16, 1, psum=True)
            hnT = B(S, ph, "hnT", [128, 16, 128], BF16, 3)
            pin = B(S, ph, "pin", [128, 256], F32, 2)
            pbf = B(S, ph, "pbf", [128, 256], BF16, 2)
            pT = B(S, ph, "pT", [128, 2, 128], BF16, 3)
            ptp2 = ph.ps("ptp2", [128, 8, 128], BF16)
            d_ptp2 = S.dep("ptp2")
            gps = B(S, ph, "gps", [128, 512], F32, 2, psum=True)
            pps = B(S, ph, "pps", [128, 512], F32, 2, psum=True)
            sgt = B(S, ph, "sgt", [128, D], F32, 3)
            h3b = B(S, ph, "h3b", [128, D], F32, 2)
            otb = B(S, ph, "otb", [128, D], F32, 2)
            def g_front(tt):
                hT, d_hT, _ = hnT.nxt()
                (xt, d_xt), = norm_transpose(ph, h2_d[tt * 128:(tt + 1) * 128, :], 1, gple[:], d_gple, xin, xn, tp, stt,
                                             lambda gt, hT=hT, d_hT=d_hT: (hT[:], d_hT), src_deps=[d_h2[tt]])
                sg, d_sg, _ = sgt.nxt()
                for c in range(4):
                    G_, d_G_, _ = gps.nxt()
                    for dc in range(16):
                        mm(G_[:], hT[:, dc, :], wpg[:, dc, c * 512:(c + 1) * 512], dc == 0, dc == 15, [d_hT, d_wpg[c]], [d_G_])
                    S.op("act", lambda e, sg=sg, G_=G_, c=c: e.activation(out=sg[:, c * 512:(c + 1) * 512], in_=G_[:], func=AF.Sigmoid), reads=[d_G_], writes=[d_sg])
                pi, d_pi, pii = pin.nxt()
                S.dma("sp", pi[:], _p_own.get()[tt * 128:(tt + 1) * 128, :], writes=[d_pi], ch=f"pin{pii}")
                pb_, d_pb, _ = pbf.nxt()
                S.op("dve", lambda e, pb_=pb_, pi=pi: e.tensor_copy(out=pb_[:], in_=pi[:]), reads=[d_pi], writes=[d_pb])
                for c2 in range(2):
                    S.op("pe", lambda e, pb_=pb_, c2=c2: e.transpose(out=ptp2[:, c2, :], in_=pb_[:, c2 * 128:(c2 + 1) * 128], identity=ident_b[:]),
                         reads=[d_pb, d_ident], writes=[d_ptp2])
                pt_, d_pt, _ = pT.nxt()
                S.op("dve", lambda e, pt_=pt_: e.tensor_copy(out=pt_[:], in_=ptp2[:, 0:2, :]), reads=[d_ptp2], writes=[d_pt])
                return (tt, xt, d_xt, sg, d_sg, pt_, d_pt)

            def g_back(tt, xt, d_xt, sg, d_sg, pt_, d_pt):
                h3, d_h3, _ = h3b.nxt()
                for c in range(4):
                    Pp, d_Pp, _ = pps.nxt()
                    for c2 in range(2):
                        mm(Pp[:], pt_[:, c2, :], wpp[:, c2, c * 512:(c + 1) * 512], c2 == 0, c2 == 1, [d_pt, d_wpp], [d_Pp])
                    S.op("dve", lambda e, h3=h3, Pp=Pp, sg=sg, c=c: e.tensor_tensor(out=h3[:, c * 512:(c + 1) * 512], in0=Pp[:], in1=sg[:, c * 512:(c + 1) * 512], op=ALU.mult),
                         reads=[d_Pp, d_sg], writes=[d_h3])
                S.op("pool", lambda e, h3=h3, xt=xt: e.tensor_tensor(out=h3[:], in0=h3[:], in1=xt[:], op=ALU.add), reads=[d_h3, d_xt], writes=[d_h3])
                ot, d_ot, oti = otb.nxt()
                st2, d_st2, _ = stt2.nxt()
                rms_norm(ph, h3[:], d_h3, gfin[:], d_gfin, ot[:], d_ot, (st2, d_st2))
                S.dma("pool", out[tt * 128:(tt + 1) * 128, :], ot[:], reads=[d_ot], writes=[d_out], ch=f"otb{oti}")

            frs = [g_front(0), g_front(1)]
            for tt in range(16):
                if tt + 2 < 16:
                    frs.append(g_front(tt + 2))
                g_back(*frs.pop(0))
    S.close()
    return nc


_PROGRAM = None


def kernel(**inputs):
    global _PROGRAM
    maps, rows_all = make_in_maps(inputs)
    if _PROGRAM is None:
        _PROGRAM = build_program()
    res = run_bass_kernel_spmd(_PROGRAM, maps, core_ids=list(range(NCORE)))
    outp = np.zeros((2, 8192, D), np.float32)
    for c, (b, rows) in enumerate(rows_all):
        outp[b][rows] = np.asarray(res.results[c]["out"]).astype(np.float32)
    return outp


def make_in_maps(inputs):
    f = lambda a: np.ascontiguousarray(np.asarray(a, dtype=np.float32))
    x = f(inputs["x"])
    p = f(inputs["p"])[0]
    ident = np.eye(128, dtype=np.float32)
    sgumask = (np.arange(128)[None, :] >= np.arange(128)[:, None]).astype(np.float32)
    iota = np.arange(128, dtype=np.float32)[None, :]
    shared = {
        "ident": ident, "sgumask": sgumask, "iota": iota,
        "norm_mix_g": f(inputs["norm_mix_g"]).reshape(1, D),
        "w_in": f(inputs["w_in"])[0],
        "sgu_ln_g": f(inputs["sgu_ln_g"]).reshape(1, 1024),
        "sgu_ln_b": f(inputs["sgu_ln_b"]).reshape(1, 1024),
        "sgu_w": f(inputs["sgu_w"])[0],
        "sgu_b": f(inputs["sgu_b"]).reshape(1, 1024),
        "w_branch_attn": f(inputs["w_branch_attn"])[0],
        "w_branch_sgu": f(inputs["w_branch_sgu"])[0],
        "w_out": f(inputs["w_out"])[0],
        "norm_ffn_g": f(inputs["norm_ffn_g"]).reshape(1, D),
        "peer_w_query": f(inputs["peer_w_query"])[0],
        "peer_sub_keys": f(inputs["peer_sub_keys"])[0].reshape(16, 128, 128),
        "peer_down": f(inputs["peer_down"])[0],
        "peer_up": f(inputs["peer_up"])[0],
        "norm_ple_g": f(inputs["norm_ple_g"]).reshape(1, D),
        "ple_w_proj": f(inputs["ple_w_proj"])[0],
        "ple_w_gate": f(inputs["ple_w_gate"])[0],
        "final_norm_g": f(inputs["final_norm_g"]).reshape(1, D),
    }
    maps = []
    rows_all = []
    for c in range(NCORE):
        b, r = c // 4, c % 4
        rows = np.concatenate([np.arange((r + 4 * i) * 256, (r + 4 * i + 1) * 256) for i in range(8)])
        rows_all.append((b, rows))
        pastb = np.zeros((8, 32), np.float32)
        ownm = np.zeros((8, 32), np.float32)
        cb = np.zeros((8, 128, 4, 2, 256), np.float32)
        for i in range(8):
            j = r + 4 * i
            pastb[i, j:] = -1e30
            ownm[i, j] = 1.0
            for hq in range(2):
                qpos = hq * 128 + np.arange(128)[:, None]
                kpos = np.arange(256)[None, :]
                cb[i, :, r, hq, :] = np.where(kpos <= qpos, 0.0, NEGB)
        m = dict(shared)
        m.update({
            "x_all": x[b], "x_own": np.ascontiguousarray(x[b][rows]), "p_own": np.ascontiguousarray(p[b][rows]),
            "pastbias": pastb.reshape(1, 256), "ownmask": ownm.reshape(1, 256),
            "cbias": cb.reshape(8, 128, 2048).astype(ml_dtypes.bfloat16),
        })
        maps.append(m)
    return maps, rows_all
```
